# Optimizing a Trainium2 kernel written in Bass

```python
import math
import jax, jax.numpy as jnp
from jax import lax
import numpy as np

D_MODEL = 1024
BATCH = 8
SEQ = 4096
DEPTH = 1

CHUNK = 64
D_MIX = D_MODEL
SSD_HEADS = 8
SSD_HEAD_DIM = 64
SSD_GROUPS = 2
SSD_STATE = 128
SSD_CONV = 4
D_SSD = SSD_HEADS * SSD_HEAD_DIM
D_XBC = D_SSD + 2 * SSD_GROUPS * SSD_STATE
FOX_HEADS = 8
FOX_HEAD_DIM = 64
D_FOX = FOX_HEADS * FOX_HEAD_DIM
Q_BLOCK = 128
PROJ_SPLITS = (D_SSD, D_SSD + D_XBC, D_SSD + D_XBC + SSD_HEADS,
               D_SSD + D_XBC + SSD_HEADS + D_FOX,
               D_SSD + D_XBC + SSD_HEADS + 2 * D_FOX,
               D_SSD + D_XBC + SSD_HEADS + 3 * D_FOX)
D_IN_PROJ = D_SSD + D_XBC + SSD_HEADS + 3 * D_FOX + FOX_HEADS
N_EXPERT_GROUPS = 4
EXPERTS_PER_GROUP = 4
TOP_K_IN_GROUP = 2
D_EXPERT = 512
D_PLE = 256
EPS = 1e-6

kernel_name = 'hymba_ssd_fox_hiermoe_ple'


def rms_norm(x, g):
    xf = x.astype(jnp.float32)
    y = xf * lax.rsqrt(jnp.mean(xf * xf, axis=-1, keepdims=True) + EPS)
    return (y * g.astype(jnp.float32)).astype(x.dtype)


def ssd_scan(xs, dt, a, bm, cm):
    b, s, G, R, P = xs.shape
    N = bm.shape[-1]
    nc = s // CHUNK
    xdt = (xs * dt[..., None]).reshape(b, nc, CHUNK, G, R, P)
    adt = (dt * a).reshape(b, nc, CHUNK, G, R).transpose(0, 3, 4, 1, 2)
    a_cs = jnp.cumsum(adt, axis=-1)
    bc = bm.reshape(b, nc, CHUNK, G, N).astype(xdt.dtype)
    cc = cm.reshape(b, nc, CHUNK, G, N).astype(xdt.dtype)
    causal = jnp.tril(jnp.ones((CHUNK, CHUNK), dtype=bool))
    seg = a_cs[..., :, None] - a_cs[..., None, :]
    decay_in = jnp.exp(jnp.where(causal, seg, -jnp.inf))
    cb = jnp.einsum('bclgn,bcsgn->bgcls', cc, bc)
    y_diag = jnp.einsum('bgcls,bgrcls,bcsgrp->bclgrp', cb, decay_in, xdt)
    decay_st = jnp.exp(a_cs[..., -1:] - a_cs)
    states = jnp.einsum('bclgn,bgrcl,bclgrp->cbgrpn', bc, decay_st, xdt)
    chunk_decay = jnp.exp(a_cs[..., -1]).transpose(3, 0, 1, 2)

    def step(carry, inp):
        st, dec = inp
        return carry * dec[..., None, None] + st, carry

    init = jnp.zeros((b, G, R, P, N), dtype=states.dtype)
    _, prev = lax.scan(step, init, (states, chunk_decay))
    y_off = jnp.einsum('bclgn,cbgrpn,bgrcl->bclgrp', cc, prev, jnp.exp(a_cs))
    return (y_diag + y_off).reshape(b, s, G, R, P)


def ssd_mixer(z, xbc, dt_raw, conv_w, conv_b, dt_bias, a_log, d_skip, g_norm):
    b, s, _ = xbc.shape
    R = SSD_HEADS // SSD_GROUPS
    xbc = lax.conv_general_dilated(xbc, conv_w, window_strides=(1,),
                                   padding=[(SSD_CONV - 1, 0)],
                                   dimension_numbers=('NWC', 'WIO', 'NWC'),
                                   feature_group_count=D_XBC) + conv_b
    xbc = jax.nn.silu(xbc)
    xs, bm, cm = jnp.split(xbc, [D_SSD, D_SSD + SSD_GROUPS * SSD_STATE], axis=-1)
    xs = xs.reshape(b, s, SSD_GROUPS, R, SSD_HEAD_DIM)
    bm = bm.reshape(b, s, SSD_GROUPS, SSD_STATE)
    cm = cm.reshape(b, s, SSD_GROUPS, SSD_STATE)
    dt = jax.nn.softplus(dt_raw.astype(jnp.float32) + dt_bias.astype(jnp.float32))
    dt = dt.reshape(b, s, SSD_GROUPS, R)
    a = -jnp.exp(a_log.astype(jnp.float32)).reshape(SSD_GROUPS, R)
    y = ssd_scan(xs, dt, a, bm, cm)
    y = y + d_skip.reshape(SSD_GROUPS, R)[:, :, None] * xs
    y = y.reshape(b, s, D_SSD)
    return rms_norm(y * jax.nn.silu(z.astype(y.dtype)), g_norm)


def fox_mixer(q, k, v, f_logit, f_bias):
    b, s, _ = q.shape
    q = q.reshape(b, s, FOX_HEADS, FOX_HEAD_DIM).transpose(0, 2, 1, 3)
    k = k.reshape(b, s, FOX_HEADS, FOX_HEAD_DIM).transpose(0, 2, 1, 3)
    v = v.reshape(b, s, FOX_HEADS, FOX_HEAD_DIM).transpose(0, 2, 1, 3)
    log_f = jax.nn.log_sigmoid((f_logit + f_bias).astype(jnp.float32))
    f_cum = jnp.cumsum(log_f, axis=1).transpose(0, 2, 1)
    scale = FOX_HEAD_DIM ** -0.5
    outs = []
    for blk in range(s // Q_BLOCK):
        q0 = blk * Q_BLOCK
        q1 = q0 + Q_BLOCK
        logits = jnp.einsum('bhqd,bhkd->bhqk', q[:, :, q0:q1], k[:, :, :q1]).astype(jnp.float32) * scale
        logits = logits + f_cum[:, :, q0:q1, None] - f_cum[:, :, None, :q1]
        mask = (q0 + jnp.arange(Q_BLOCK))[:, None] >= jnp.arange(q1)[None, :]
        probs = jax.nn.softmax(jnp.where(mask, logits, -jnp.inf), axis=-1)
        outs.append(jnp.einsum('bhqk,bhkd->bhqd', probs.astype(v.dtype), v[:, :, :q1]))
    o = jnp.concatenate(outs, axis=2)
    return o.transpose(0, 2, 1, 3).reshape(b, s, D_FOX)


def hier_moe(h, w_rg, b_rg, w_re, b_re, w_gate, w_up, w_down):
    b, s, _ = h.shape
    grp_prob = jax.nn.softmax((h @ w_rg + b_rg).astype(jnp.float32), axis=-1)
    p_grp, g_idx = lax.top_k(grp_prob, 1)
    grp_onehot = jax.nn.one_hot(g_idx[..., 0], N_EXPERT_GROUPS, dtype=jnp.float32)
    exp_logits = (h @ w_re + b_re).astype(jnp.float32).reshape(b, s, N_EXPERT_GROUPS, EXPERTS_PER_GROUP)
    in_grp = jnp.einsum('bsg,bsge->bse', grp_onehot, exp_logits)
    top_val, top_idx = lax.top_k(in_grp, TOP_K_IN_GROUP)
    w_top = jax.nn.softmax(top_val, axis=-1) * p_grp
    w_exp = jnp.sum(jax.nn.one_hot(top_idx, EXPERTS_PER_GROUP, dtype=jnp.float32) * w_top[..., None], axis=-2)
    combine = (grp_onehot[..., :, None] * w_exp[..., None, :]).astype(h.dtype)
    y = jnp.zeros_like(h)
    for g in range(N_EXPERT_GROUPS):
        act = jax.nn.silu(jnp.einsum('bsd,edf->bsef', h, w_gate[g]))
        hid = act * jnp.einsum('bsd,edf->bsef', h, w_up[g]) * combine[:, :, g, :, None]
        y = y + jnp.einsum('bsef,efd->bsd', hid, w_down[g])
    return y


def setup_inputs(seed: int = 0) -> dict:
    key = jax.random.key(seed)
    ks = jax.random.split(key, 24)
    nrm = jax.random.normal
    L = DEPTH
    x = nrm(ks[0], (BATCH, SEQ, D_MODEL), jnp.float32)
    p = nrm(ks[1], (L, BATCH, SEQ, D_PLE), jnp.float32)
    w_in = nrm(ks[2], (L, D_MODEL, D_IN_PROJ), jnp.float32) * D_MODEL ** -0.5
    conv_w = nrm(ks[3], (L, SSD_CONV, 1, D_XBC), jnp.float32) * SSD_CONV ** -0.5
    conv_b = 0.01 * nrm(ks[4], (L, D_XBC), jnp.float32)
    u = jax.random.uniform(ks[5], (L, SSD_HEADS), jnp.float32)
    dt0 = jnp.exp(u * (math.log(0.1) - math.log(0.001)) + math.log(0.001))
    dt_bias = dt0 + jnp.log(-jnp.expm1(-dt0))
    a_log = jnp.log(jax.random.uniform(ks[6], (L, SSD_HEADS), jnp.float32, 1.0, 16.0))
    d_skip = 1.0 + 0.1 * nrm(ks[7], (L, SSD_HEADS), jnp.float32)
    g_ssd = 1.0 + 0.05 * nrm(ks[8], (L, D_SSD), jnp.float32)
    fox_fbias = 2.0 + 0.5 * nrm(ks[9], (L, FOX_HEADS), jnp.float32)
    w_out = nrm(ks[10], (L, D_MIX, D_MODEL), jnp.float32) * D_MIX ** -0.5
    g_mix = 1.0 + 0.05 * nrm(ks[11], (L, D_MODEL), jnp.float32)
    g_ffn = 1.0 + 0.05 * nrm(ks[12], (L, D_MODEL), jnp.float32)
    w_route_group = nrm(ks[13], (L, D_MODEL, N_EXPERT_GROUPS), jnp.float32) * D_MODEL ** -0.5
    b_route_group = 0.01 * nrm(ks[14], (L, N_EXPERT_GROUPS), jnp.float32)
    w_route_expert = nrm(ks[15], (L, D_MODEL, N_EXPERT_GROUPS * EXPERTS_PER_GROUP), jnp.float32) * D_MODEL ** -0.5
    b_route_expert = 0.01 * nrm(ks[16], (L, N_EXPERT_GROUPS * EXPERTS_PER_GROUP), jnp.float32)
    eshape = (L, N_EXPERT_GROUPS, EXPERTS_PER_GROUP, D_MODEL, D_EXPERT)
    w_exp_gate = nrm(ks[17], eshape, jnp.float32) * D_MODEL ** -0.5
    w_exp_up = nrm(ks[18], eshape, jnp.float32) * D_MODEL ** -0.5
    w_exp_down = nrm(ks[19], (L, N_EXPERT_GROUPS, EXPERTS_PER_GROUP, D_EXPERT, D_MODEL), jnp.float32) * D_EXPERT ** -0.5
    g_ple = 1.0 + 0.05 * nrm(ks[20], (L, D_MODEL), jnp.float32)
    w_ple_proj = nrm(ks[21], (L, D_PLE, D_MODEL), jnp.float32) * D_PLE ** -0.5
    w_ple_gate = nrm(ks[22], (L, D_MODEL, D_MODEL), jnp.float32) * D_MODEL ** -0.5
    g_final = 1.0 + 0.05 * nrm(ks[23], (D_MODEL,), jnp.float32)
    return {'x': x, 'p': p, 'w_in': w_in, 'conv_w': conv_w, 'conv_b': conv_b,
            'dt_bias': dt_bias, 'a_log': a_log, 'd_skip': d_skip, 'g_ssd': g_ssd,
            'fox_fbias': fox_fbias, 'w_out': w_out, 'g_mix': g_mix, 'g_ffn': g_ffn,
            'w_route_group': w_route_group, 'b_route_group': b_route_group,
            'w_route_expert': w_route_expert, 'b_route_expert': b_route_expert,
            'w_exp_gate': w_exp_gate, 'w_exp_up': w_exp_up, 'w_exp_down': w_exp_down,
            'g_ple': g_ple, 'w_ple_proj': w_ple_proj, 'w_ple_gate': w_ple_gate,
            'g_final': g_final}


def reference(x, p, w_in, conv_w, conv_b, dt_bias, a_log, d_skip, g_ssd, fox_fbias,
              w_out, g_mix, g_ffn, w_route_group, b_route_group, w_route_expert,
              b_route_expert, w_exp_gate, w_exp_up, w_exp_down, g_ple, w_ple_proj,
              w_ple_gate, g_final):
    h = x
    for i in range(DEPTH):
        hn = rms_norm(h, g_mix[i])
        proj = hn @ w_in[i]
        z, xbc, dt_raw, q, k, v, f_logit = jnp.split(proj, PROJ_SPLITS, axis=-1)
        y_ssd = ssd_mixer(z, xbc, dt_raw, conv_w[i], conv_b[i], dt_bias[i],
                          a_log[i], d_skip[i], g_ssd[i])
        y_fox = fox_mixer(q, k, v, f_logit, fox_fbias[i])
        mixed = jnp.concatenate([y_ssd.astype(h.dtype), y_fox.astype(h.dtype)], axis=-1)
        h = h + mixed @ w_out[i]
        h = h + hier_moe(rms_norm(h, g_ffn[i]), w_route_group[i], b_route_group[i],
                         w_route_expert[i], b_route_expert[i], w_exp_gate[i],
                         w_exp_up[i], w_exp_down[i])
        gate = jax.nn.sigmoid((rms_norm(h, g_ple[i]) @ w_ple_gate[i]).astype(jnp.float32))
        h = h + ((p[i] @ w_ple_proj[i]).astype(jnp.float32) * gate).astype(h.dtype)
    return rms_norm(h, g_final)
```

```python
import numpy as np
import concourse.bass as bass
import concourse.mybir as mybir
from concourse.bass_utils import run_bass_kernel_spmd
from contextlib import ExitStack

F32 = mybir.dt.float32
BF16 = mybir.dt.bfloat16
AF = mybir.ActivationFunctionType
ALU = mybir.AluOpType

ENGS = ('pe', 'act', 'dve', 'pool', 'sp')
_CACHE = {}
OPT = {'la': 2, 'prefA': True, 'rawact': True}
NDS = 40

S_TOK = 4096
D = 1024
TB = 256
NB = S_TOK // TB
T2 = 1024
EPS = 1e-6
NEG = -30000.0

P_FB, P_DTB, P_ALOG, P_DSK, P_GMIX, P_GFFN, P_GPLE, P_CB, P_CW, P_BR, P_GSSD, P_GFIN, P_W = \
    0, 8, 16, 24, 32, 40, 48, 56, 64, 96, 128, 640, 1664
C_ID, C_TRI, C_ONE, C_NEG, C_BM, C_W = 0, 128, 256, 384, 512, 1536


class Op:
    __slots__ = ('eng', 'fn', 'deps', 'dma', 'ds', 'dval', 'sig', 'cnt', 'idx')


class _Rec:
    def __getattr__(self, name):
        def f(*a, **k):
            self.call = (name, a, k)
            return self
        return f


class Sched:
    def __init__(self, nc, es):
        self.nc = nc
        self.sem = {e: es.enter_context(nc.semaphore("s_" + e)) for e in ENGS}
        self.dsem = [es.enter_context(nc.semaphore("d%d" % i)) for i in range(NDS)]
        self.dval = [0] * NDS
        self.dlast = [None] * NDS
        self.dnext = {'pool': 0, 'sp': NDS // 2, 'act': NDS // 2}
        self.cnt = {e: 0 for e in ENGS}
        self.ops = []
        self.lastw = {}
        self.readers = {}
        self.nops = 0
        self.last_fp32 = None

    def add(self, eng, fn, r=(), w=(), dma=False):
        o = Op()
        isfp32 = False
        if fn is not None:
            rec = _Rec()
            fn(rec)
            c0 = rec.call
            if eng == 'pe' and c0[0] == 'matmul':
                lhsT = c0[1][1] if len(c0[1]) > 1 else c0[2].get('lhsT')
                isfp32 = (lhsT.dtype == F32)
            fn = (lambda e, c=rec.call: getattr(e, c[0])(*c[1], **c[2]))
        o.eng = eng; o.fn = fn; o.dma = dma; o.sig = False; o.cnt = None
        o.idx = len(self.ops)
        deps = []
        if eng == 'pe' and OPT.get('fp32fence', True) and fn is not None:
            lp = self.last_fp32
            if lp is not None and lp[1] != isfp32:
                deps.append((lp[0], True))
            self.last_fp32 = (o, isfp32)
        for k in r:
            lw = self.lastw.get(k)
            if lw is not None:
                deps.append((lw, True))
        for k in w:
            lw = self.lastw.get(k)
            if lw is not None:
                deps.append((lw, False))
            for rd in self.readers.get(k, ()):
                deps.append((rd, False))
        if dma:
            j = self.dnext[eng]
            half = NDS // 2
            base = 0 if eng == 'pool' else half
            nxt = base + (j - base + 1) % half
            for e2 in self.dnext:
                if (e2 == 'pool') == (eng == 'pool'):
                    self.dnext[e2] = nxt
            o.ds = j
            self.dval[j] += 16
            o.dval = self.dval[j]
            if self.dlast[j] is not None:
                deps.append((self.dlast[j], True))
            self.dlast[j] = o
        else:
            o.ds = None; o.dval = None
        dd = {}
        for d, raw in deps:
            if d is o:
                continue
            if d.dma or d.eng != eng or raw or eng != 'pe':
                dd[id(d)] = d
        o.deps = list(dd.values())
        for k in r:
            self.readers.setdefault(k, []).append(o)
        for k in w:
            self.lastw[k] = o
            self.readers[k] = []
        self.ops.append(o)
        return o

    def pe(self, fn, r=(), w=()): return self.add('pe', fn, r, w)
    def act(self, fn, r=(), w=()): return self.add('act', fn, r, w)
    def dve(self, fn, r=(), w=()): return self.add('dve', fn, r, w)
    def pool(self, fn, r=(), w=()): return self.add('pool', fn, r, w)

    def dma(self, eng, out, in_, r=(), w=()):
        return self.add(eng, lambda e: e.dma_start(out=out, in_=in_), r, w, dma=True)

    def emit(self):
        nc = self.nc
        ops = self.ops
        for o in ops:
            for d in o.deps:
                if not d.dma:
                    d.sig = True
        last = {}
        for o in ops:
            if not o.dma and o.fn is not None:
                last[o.eng] = o
        for o in last.values():
            o.sig = True
        cnt = dict(self.cnt)
        for o in ops:
            if not o.dma and o.sig:
                cnt[o.eng] += 1
                o.cnt = cnt[o.eng]
        endcnt = cnt
        per = {e: [o for o in ops if o.eng == e] for e in ENGS}
        sem = self.sem; dsem = self.dsem
        dval_end = list(self.dval)

        def body(ename):
            def run(eng):
                known = {}
                for o in per[ename]:
                    for d in o.deps:
                        if d.dma:
                            key = ('d', d.ds); val = d.dval; s = dsem[d.ds]
                        else:
                            key = d.eng; val = d.cnt; s = sem[d.eng]
                        if known.get(key, 0) >= val:
                            continue
                        known[key] = val
                        eng.wait_ge(s, val)
                    if o.fn is None:
                        continue
                    ins = o.fn(eng)
                    if o.dma:
                        ins.then_inc(dsem[o.ds], 16)
                    elif o.sig:
                        ins.then_inc(sem[ename], 1)
                for f in ENGS:
                    if f != ename and endcnt[f] > 0:
                        eng.wait_ge(sem[f], endcnt[f])
                for j in range(NDS):
                    if dval_end[j] > 0:
                        eng.wait_ge(dsem[j], dval_end[j])
            return run

        with nc.Block() as block:
            block.tensor(body('pe'))
            block.scalar(body('act'))
            block.vector(body('dve'))
            block.gpsimd(body('pool'))
            block.sync(body('sp'))
        self.cnt = endcnt
        self.nops += len(ops)
        self.ops = []
        self.lastw = {}
        self.readers = {}
        self.dlast = [None] * NDS
        self.last_fp32 = None


def build(stage=3, nb=NB, parts=15, nb2=S_TOK // T2, nexp=16, p1=True):
    nc = bass.Bass("TRN2", target_bir_lowering=False)

    def din(name, shape):
        return nc.dram_tensor(name, shape, F32, kind="ExternalInput").ap()

    x = din("x", [S_TOK, D])
    p_in = din("p", [S_TOK, 256])
    w_in = din("w_in", [D, 3088])
    w_sm = din("w_sm", [D, 16])
    w_out = din("w_out", [D, D])
    w_rt = din("w_rt", [D, 20])
    w_g = din("w_g", [16, D, 512])
    w_u = din("w_u", [16, D, 512])
    w_d = din("w_d", [16, 512, D])
    w_pg = din("w_pg", [D, D])
    w_pp = din("w_pp", [256, D])
    pbd = din("pb", [128, P_W])
    cstd = din("cst", [128, C_W])
    out = nc.dram_tensor("out", [S_TOK, D], F32, kind="ExternalOutput").ap()
    if p1:
        h1d = nc.dram_tensor("h1d", [S_TOK, D], F32, kind="Internal").ap()
    else:
        h1d = din("h1dbg", [S_TOK, D])

    with ExitStack() as es0:
        S = Sched(nc, es0)

        with ExitStack() as es:
            if not p1:
                nb = 0
            def sb(name, shape, dt=F32):
                return es.enter_context(nc.sbuf_tensor("t_" + name, shape, dt))

            def psb(name):
                return es.enter_context(nc.psum_tensor(name, [128, 512], F32))

            B = [psb("B%d" % i) for i in range(8)]
            B0b = B[0][:].bitcast(BF16)
            B1b = B[1][:].bitcast(BF16)

            cst = sb("cst", [128, C_W - 128])
            pb = sb("pb", [128, P_GFIN])
            ident = sb("ident", [128, 128], BF16)
            negm = sb("negm", [128, 512], BF16)
            wsm = sb("wsm", [128, 8, 16], BF16)
            a_b = sb("a_b", [128, 8])
            KT = [sb("KT%d" % h, [96, S_TOK], BF16) for h in range(8)]
            Vx = sb("Vx", [128, 32, 8, 65], BF16)
            Fk = sb("Fk", [128, 32, 8])
            carF = sb("carF", [128, 8])
            carFT = sb("carFT", [8, 1])
            S32 = sb("S32", [128, 512])
            Sbf = sb("Sbf", [128, 512], BF16)
            halo = sb("halo", [128, 8, 3])
            wbuf = [sb("wbuf%d" % i, [128, 4096], BF16) for i in range(3)]
            xt = [sb("xt%d" % i, [128, D]) for i in range(2)]
            xn2 = [sb("xn2%d" % i, [128, D], BF16) for i in range(2)]
            xr = [sb("xr%d" % i, [128, D]) for i in range(2)]
            ssqA = [sb("ssqA%d" % i, [128, 1]) for i in range(2)]; rstdA = [sb("rstdA%d" % i, [128, 1]) for i in range(2)]
            Osb8all = sb("Osb8all", [65, 4 * TB])
            ssq2 = sb("ssq2", [128, 1]); rstd2 = sb("rstd2", [128, 1])
            hnT = sb("hnT", [128, 8, TB], BF16)
            raw = [sb("raw%d" % i, [128, TB + 3]) for i in range(2)]
            cacc = [sb("cacc%d" % i, [128, TB]) for i in range(2)]
            xbcT = sb("xbcT", [128, 8, TB], BF16)
            zs = [sb("zs%d" % i, [128, 512], BF16) for i in range(2)]
            QT = [sb("QT%d" % h, [96, TB], BF16) for h in range(8)]
            tmpc = []
            for tt_ in range(2):
                tmpc.append({
                    'sm': sb("sm%d" % tt_, [128, 16]), 'sp': sb("sp%d" % tt_, [128, 16]), 'dt_t': sb("dt_t%d" % tt_, [128, 8]),
                    'ea': sb("ea%d" % tt_, [128, 8]), 'cd_b': sb("cd_b%d" % tt_, [128, 8]), 'tot_sb': sb("tot_sb%d" % tt_, [128, 8]),
                    'dd': sb("dd%d" % tt_, [128, 8]), 'dst': sb("dst%d" % tt_, [128, 8]),
                    'nacsT': sb("nacsT%d" % tt_, [96, 128]), 'BDacs': sb("BDacs%d" % tt_, [96, 1024])})
            rTb = sb("rTb", [8, TB], BF16)
            xsB = sb("xsB", [128, 768], BF16)
            xdt = sb("xdt", [128, 512], BF16); xsD = sb("xsD", [128, 512], BF16); xdtd = sb("xdtd", [128, 512], BF16)
            dec1 = sb("dec1", [128, 512]); dec = [dec1, dec1]; MT = [sb("MT%d" % g, [128, 512], BF16) for g in range(2)]
            y1 = sb("y1", [128, 512]); gy = sb("gy", [128, 512]); yn = sb("yn", [128, 512], BF16)
            mixT = sb("mixT", [128, 4, TB], BF16)
            PT = [sb("PT%d" % i, [128, TB], BF16) for i in range(3)]
            yTall = sb("yTall", [96, 8, TB], BF16)

            S.dma('sp', cst[:], cstd[:, 128:C_W], w=['cst'])
            S.dma('sp', pb[:], pbd[:, 0:P_GFIN], w=['pb'])
            S.dma('pool', wsm[:], w_sm.rearrange("(kc p) c -> p kc c", p=128), w=['wsm'])
            S.dma('pool', ident[:], cstd[:, C_ID:C_ID + 128], w=['ident'])
            for i in range(4):
                S.dve(lambda e, i=i: e.tensor_copy(out=negm[:, i * 128:(i + 1) * 128], in_=cst[:, C_NEG - 128:C_NEG]),
                      r=['cst'], w=['negm'])
            tri = cst[:, C_TRI - 128:C_TRI]
            ones = cst[:, C_ONE - 128:C_ONE]
            bmask = cst[0:8, C_BM - 128:C_BM - 128 + 1024]
            bmask96 = cst[0:96, C_BM - 128:C_BM - 128 + 1024]
            S.act(lambda e: e.activation(out=a_b[:], in_=pb[:, P_ALOG:P_ALOG + 8], func=AF.Exp), r=['pb'], w=['a_b'])
            S.dve(lambda e: e.tensor_scalar(out=a_b[:], in0=a_b[:], scalar1=-1.0, scalar2=None, op0=ALU.mult),
                  r=['a_b'], w=['a_b'])
            for h in range(8):
                S.pool(lambda e, h=h: e.memset(KT[h][64:96, :], 0.0), w=['KT%d' % h])
                S.pool(lambda e, h=h: e.memset(KT[h][64:65, :], 1.0), w=['KT%d' % h])
                S.pool(lambda e, h=h: e.memset(QT[h][64:96, :], 0.0), w=['QT%d' % h])
            S.pool(lambda e: e.memset(Vx[:], 1.0), w=['Vx'])
            S.pool(lambda e: e.memset(carF[:], 0.0), w=['carF'])
            S.pool(lambda e: e.memset(carFT[:], 0.0), w=['carFT'])
            S.pool(lambda e: e.memset(S32[:], 0.0), w=['S32'])
            S.pool(lambda e: e.memset(Sbf[:], 0.0), w=['Sbf'])
            S.pool(lambda e: e.memset(halo[:], 0.0), w=['halo'])
            for tt_ in range(2):
                S.pool(lambda e: e.memset(tmpc[tt_]['nacsT'][:], 0.0), w=['nacsT_%d' % tt_])
                S.pool(lambda e: e.memset(tmpc[tt_]['BDacs'][:], 0.0), w=['BDacs_%d' % tt_])
            S.pool(lambda e: e.memset(yTall[64:96, :, :], 0.0), w=['yT'])

            wstate = {'n': 0}

            def wsrc(ci):
                c = ci % 9
                if c < 6:
                    c0 = [0, 512, 1024, 1544, 2056, 2568][c]
                    return (128, w_in[:, c0:c0 + 512].rearrange("(kc p) c -> p kc c", p=128), [128, 8, 512])
                if c == 6:
                    return (128, w_out[0:512, :].rearrange("(kc p) c -> p kc c", p=128), [128, 4, 1024])
                hh = (c - 7) * 4
                return (64, w_out[512 + hh * 64:512 + (hh + 4) * 64, :].rearrange("(h p) c -> p h c", p=64), [64, 4, 1024])

            def wload():
                ci = wstate['n']
                if ci >= NB * 9:
                    return
                wstate['n'] += 1
                bi = ci % 3
                npart, src, shp = wsrc(ci)
                dstv = wbuf[bi][0:npart, :].rearrange("p (a b) -> p a b", a=shp[1])
                S.dma('pool', dstv, src, w=['wbuf%d' % bi])

            def wview(ci):
                bi = ci % 3
                npart, src, shp = wsrc(ci)
                return wbuf[bi][0:npart, :].rearrange("p (a b) -> p a b", a=shp[1]), 'wbuf%d' % bi

            wload(); wload(); wload()

            def secA_pre(bb):
                for tt in range(2):
                    r0 = bb * TB + tt * 128
                    X = xt[tt]; kx = 'xt%d' % tt
                    S.dma('sp', X[:], x[r0:r0 + 128, :], w=[kx])
                    S.act(lambda e: e.activation(out=xn2[tt][:], in_=X[:], func=AF.Square, accum_out=ssqA[tt][:]),
                          r=[kx], w=['xn2%d' % tt, 'ssqA%d' % tt])
                    S.act(lambda e: e.activation(out=rstdA[tt][:], in_=ssqA[tt][:], func=AF.Sqrt, scale=1.0 / D, bias=EPS),
                          r=['ssqA%d' % tt], w=['rstdA%d' % tt])
                    S.dve(lambda e: e.reciprocal(out=rstdA[tt][:], in_=rstdA[tt][:]), r=['rstdA%d' % tt], w=['rstdA%d' % tt])
                    S.pool(lambda e: e.tensor_scalar(out=xn2[tt][:], in0=X[:], scalar1=rstdA[tt][:, 0:1], scalar2=1.0,
                                                     op0=ALU.mult, op1=ALU.mult), r=[kx, 'rstdA%d' % tt], w=['xn2%d' % tt])

            def secA_pe(bb):
                for tt in range(2):
                    Bt = (B1b, B0b)[tt]; kb = ('B1', 'B0')[tt]
                    for kc in range(8):
                        S.pe(lambda e, kc=kc: e.transpose(Bt[:, kc * 128:(kc + 1) * 128], xn2[tt][:, kc * 128:(kc + 1) * 128], ident[:]),
                             r=['xn2%d' % tt, 'ident'], w=[kb])
                    S.dve(lambda e: e.tensor_tensor(
                        out=hnT[:, :, tt * 128:(tt + 1) * 128],
                        in0=Bt.rearrange("p (a b) -> p a b", a=8),
                        in1=pb[:, P_GMIX:P_GMIX + 8].unsqueeze(2).to_broadcast([128, 8, 128]), op=ALU.mult),
                        r=[kb, 'pb'], w=['hnT'])

            for b in range(nb):
                t0 = b * TB
                if b == 0 or not OPT['prefA']:
                    secA_pre(b); secA_pe(b)
                for tt in range(2):
                    r0 = t0 + tt * 128
                    S.dma('sp', xr[tt][:], x[r0:r0 + 128, :], w=['xr%d' % tt])

                ci0 = b * 9
                for tt in range(2):
                    for kc in range(8):
                        S.pe(lambda e, tt=tt, kc=kc: e.matmul(B[4][:, 288 + tt * 16:304 + tt * 16], hnT[:, kc, tt * 128:(tt + 1) * 128],
                                                              wsm[:, kc, :], start=(kc == 0), stop=(kc == 7)),
                             r=['hnT', 'wsm'], w=['B4'])
                wz, kz = wview(ci0 + 0)
                for tt in range(2):
                    pz = B[2 + tt]; kp = 'B%d' % (2 + tt)
                    for kc in range(8):
                        S.pe(lambda e, tt=tt, kc=kc, pz=pz: e.matmul(pz[:, :], hnT[:, kc, tt * 128:(tt + 1) * 128], wz[:, kc, :],
                                                                   start=(kc == 0), stop=(kc == 7)),
                             r=['hnT', kz], w=[kp])
                    S.act(lambda e, tt=tt, pz=pz: e.activation(out=zs[tt][:], in_=pz[:, :], func=AF.Silu),
                          r=[kp], w=['zs%d' % tt])
                for tt in range(2):
                    kt = 2 * b + tt
                    tk = slice(tt * 128, (tt + 1) * 128)
                    psm = B[4][:, 288 + tt * 16:304 + tt * 16]
                    T = tmpc[tt]
                    sm, sp_, dt_t, ea, cd_b, tot_sb, dd, dst, nacsT, BDacs = (T[k] for k in
                        ('sm', 'sp', 'dt_t', 'ea', 'cd_b', 'tot_sb', 'dd', 'dst', 'nacsT', 'BDacs'))
                    K_ = lambda n, tt=tt: '%s_%d' % (n, tt)
                    S.dve(lambda e: e.scalar_tensor_tensor(out=sm[:, 0:8], in0=psm[:, 0:8], scalar=-1.0,
                                                           in1=pb[:, P_FB:P_FB + 8], op0=ALU.mult, op1=ALU.subtract),
                          r=['B4', 'pb'], w=[K_('sm')])
                    S.dve(lambda e: e.tensor_tensor(out=sm[:, 8:16], in0=psm[:, 8:16], in1=pb[:, P_DTB:P_DTB + 8], op=ALU.add),
                          r=['B4', 'pb'], w=[K_('sm')])
                    S.act(lambda e: e.activation(out=sp_[:], in_=sm[:], func=AF.Exp), r=[K_('sm')], w=[K_('sp')])
                    S.act(lambda e: e.activation(out=sp_[:], in_=sp_[:], func=AF.Ln, bias=1.0), r=[K_('sp')], w=[K_('sp')])
                    S.dve(lambda e: e.tensor_copy(out=dt_t[:], in_=sp_[:, 8:16]), r=[K_('sp')], w=[K_('dt_t')])
                    S.dve(lambda e: e.tensor_tensor(out=sp_[:, 8:16], in0=sp_[:, 8:16], in1=a_b[:], op=ALU.mult),
                          r=[K_('sp'), 'a_b'], w=[K_('sp')])
                    S.pe(lambda e: e.matmul(B[4][:, 0:16], tri, sp_[:], start=True, stop=True), r=[K_('sp'), 'cst'], w=['B4'])
                    S.pe(lambda e: e.matmul(B[4][:, 16:32], ones, sp_[:], start=True, stop=True), r=[K_('sp'), 'cst'], w=['B4'])
                    S.pe(lambda e: e.matmul(B[4][0:8, 32:160], sp_[:, 8:16], tri, start=True, stop=True), r=[K_('sp'), 'cst'], w=['B4'])
                    S.pe(lambda e: e.matmul(B[4][0:8, 160:288], sp_[:, 0:8], tri, start=True, stop=True), r=[K_('sp'), 'cst'], w=['B4'])
                    S.dve(lambda e: e.tensor_tensor(out=Fk[:, kt, :], in0=B[4][:, 0:8], in1=carF[:], op=ALU.add),
                          r=['B4', 'carF'], w=['Fk'])
                    S.dve(lambda e: e.tensor_scalar(out=rTb[:, tk], in0=B[4][0:8, 160:288], scalar1=carFT[0:8, 0:1],
                                                    scalar2=-1.0, op0=ALU.add, op1=ALU.mult),
                          r=['B4', 'carFT'], w=['rTb'])
                    S.dve(lambda e: e.tensor_tensor(out=carF[:], in0=B[4][:, 16:24], in1=carF[:], op=ALU.add),
                          r=['B4', 'carF'], w=['carF'])
                    S.dve(lambda e: e.tensor_tensor(out=carFT[:], in0=B[4][0:8, 287:288], in1=carFT[:], op=ALU.add),
                          r=['B4', 'carFT'], w=['carFT'])
                    S.act(lambda e: e.activation(out=ea[:], in_=B[4][:, 8:16], func=AF.Exp), r=['B4'], w=[K_('ea')])
                    S.act(lambda e: e.activation(out=cd_b[:], in_=B[4][:, 24:32], func=AF.Exp), r=['B4'], w=[K_('cd_b')])
                    S.dve(lambda e: e.tensor_copy(out=tot_sb[:], in_=B[4][:, 24:32]), r=['B4'], w=[K_('tot_sb')])
                    S.dve(lambda e: e.scalar_tensor_tensor(out=dd[:], in0=B[4][:, 8:16], scalar=-1.0, in1=tot_sb[:],
                                                           op0=ALU.mult, op1=ALU.add), r=['B4', K_('tot_sb')], w=[K_('dd')])
                    S.act(lambda e: e.activation(out=dst[:], in_=dd[:], func=AF.Exp), r=[K_('dd')], w=[K_('dst')])
                    S.dve(lambda e: e.tensor_scalar(out=nacsT[0:8, :], in0=B[4][0:8, 32:160], scalar1=-1.0, scalar2=None, op0=ALU.mult),
                          r=['B4'], w=[K_('nacsT')])
                    S.dve(lambda e: e.tensor_tensor(out=BDacs[0:8, :].rearrange("k (r l) -> k r l", r=8),
                                                    in0=B[4][0:8, 32:160].unsqueeze(1).to_broadcast([8, 8, 128]),
                                                    in1=bmask.rearrange("k (r l) -> k r l", r=8), op=ALU.mult),
                          r=['B4', 'cst'], w=[K_('BDacs')])
                for h in range(8):
                    S.dma('sp', QT[h][64:65, :], rTb[h:h + 1, :], r=['rTb'], w=['QT%d' % h])

                wload()
                pend_silu = []
                for fc in range(8):
                    wx, kw = wview(ci0 + 1 + fc // 4)
                    pbk = B[(2, 3, 6, 7)[fc % 4]]; kp = 'B%d' % (2, 3, 6, 7)[fc % 4]
                    R = raw[fc % 2]; kr = 'raw%d' % (fc % 2)
                    A = cacc[fc % 2]; ka = 'cacc%d' % (fc % 2)
                    for kc in range(8):
                        S.pe(lambda e, fc=fc, kc=kc, pbk=pbk, wx=wx: e.matmul(
                            pbk[:, 0:TB], wx[:, kc, (fc % 4) * 128:(fc % 4 + 1) * 128], hnT[:, kc, :],
                            start=(kc == 0), stop=(kc == 7)), r=['hnT', kw], w=[kp])
                    if fc == 3:
                        wload()
                    S.pool(lambda e, fc=fc, R=R: e.tensor_copy(out=R[:, 0:3], in_=halo[:, fc, :]), r=['halo'], w=[kr + 'h'])
                    if OPT['rawact']:
                        S.act(lambda e, pbk=pbk, R=R: e.activation(out=R[:, 3:3 + TB], in_=pbk[:, 0:TB], func=AF.Copy),
                              r=[kp], w=[kr])
                    else:
                        S.dve(lambda e, pbk=pbk, R=R: e.tensor_copy(out=R[:, 3:3 + TB], in_=pbk[:, 0:TB]),
                              r=[kp], w=[kr])
                    S.pool(lambda e, fc=fc, R=R: e.tensor_copy(out=halo[:, fc, :], in_=R[:, TB:TB + 3]), r=[kr], w=['halo'])
                    for (fc_, A_, ka_) in pend_silu:
                        S.act(lambda e: e.activation(out=xbcT[:, fc_, :], in_=A_[:], func=AF.Silu), r=[ka_], w=['xbcT'])
                    pend_silu = []
                    cw0 = P_CW + fc * 4
                    S.dve(lambda e, R=R, A=A, cw0=cw0, fc=fc: e.tensor_scalar(
                        out=A[:], in0=R[:, 0:TB], scalar1=pb[:, cw0:cw0 + 1], scalar2=pb[:, P_CB + fc:P_CB + fc + 1],
                        op0=ALU.mult, op1=ALU.add), r=[kr, kr + 'h', 'pb'], w=[ka])
                    for k in range(1, 4):
                        S.dve(lambda e, R=R, A=A, cw0=cw0, k=k: e.scalar_tensor_tensor(
                            out=A[:], in0=R[:, k:k + TB], scalar=pb[:, cw0 + k:cw0 + k + 1], in1=A[:],
                            op0=ALU.mult, op1=ALU.add), r=[kr, kr + 'h', ka, 'pb'], w=[ka])
                    pend_silu.append((fc, A, ka))
                for (fc_, A_, ka_) in pend_silu:
                    S.act(lambda e: e.activation(out=xbcT[:, fc_, :], in_=A_[:], func=AF.Silu), r=[ka_], w=['xbcT'])
                wload()
                wq, kwq = wview(ci0 + 3)
                wk, kwk = wview(ci0 + 4)
                for h in range(8):
                    pbk = B[(2, 3, 6, 7)[h % 4]]; kp = 'B%d' % (2, 3, 6, 7)[h % 4]
                    for kc in range(8):
                        S.pe(lambda e, h=h, kc=kc, pbk=pbk: e.matmul(pbk[0:64, 0:TB], wq[:, kc, h * 64:(h + 1) * 64], hnT[:, kc, :],
                                                                   start=(kc == 0), stop=(kc == 7)),
                             r=['hnT', kwq], w=[kp])
                    S.dve(lambda e, h=h, pbk=pbk: e.tensor_scalar(out=QT[h][0:64, :], in0=pbk[0:64, 0:TB], scalar1=0.125,
                                                                  scalar2=None, op0=ALU.mult),
                          r=[kp], w=['QT%d' % h])
                wload()
                for h in range(8):
                    pbk = B[(2, 3, 6, 7)[h % 4]]; kp = 'B%d' % (2, 3, 6, 7)[h % 4]
                    for kc in range(8):
                        S.pe(lambda e, h=h, kc=kc, pbk=pbk: e.matmul(pbk[0:64, 0:TB], wk[:, kc, h * 64:(h + 1) * 64], hnT[:, kc, :],
                                                                   start=(kc == 0), stop=(kc == 7)),
                             r=['hnT', kwk], w=[kp])
                    S.dve(lambda e, h=h, pbk=pbk: e.tensor_copy(out=KT[h][0:64, t0:t0 + TB], in_=pbk[0:64, 0:TB]),
                          r=[kp], w=['KT%d' % h])
                wload()
                wv, kwv = wview(ci0 + 5)
                for tt in range(2):
                    pv = B[2 + tt]; kp = 'B%d' % (2 + tt)
                    kt = 2 * b + tt
                    for kc in range(8):
                        S.pe(lambda e, tt=tt, kc=kc, pv=pv: e.matmul(pv[:, :], hnT[:, kc, tt * 128:(tt + 1) * 128], wv[:, kc, :],
                                                                   start=(kc == 0), stop=(kc == 7)),
                             r=['hnT', kwv], w=[kp])
                    S.dve(lambda e, kt=kt, pv=pv: e.tensor_copy(out=Vx[:, kt, :, 0:64],
                                                                in_=pv[:, :].rearrange("p (h d) -> p h d", h=8)),
                          r=[kp], w=['Vx'])
                wload()

                def ssd_gen():
                    for tt in range(2):
                        tk = slice(tt * 128, (tt + 1) * 128)
                        T = tmpc[tt]
                        dt_t, ea, cd_b, dst, nacsT, BDacs = (T[k] for k in ('dt_t', 'ea', 'cd_b', 'dst', 'nacsT', 'BDacs'))
                        K_ = lambda n, tt=tt: '%s_%d' % (n, tt)
                        for fc in range(6):
                            S.pe(lambda e, fc=fc: e.transpose(B1b[:, fc * 128:(fc + 1) * 128], xbcT[:, fc, tk], ident[:]),
                                 r=['xbcT', 'ident'], w=['B1'])
                        S.dve(lambda e: e.tensor_copy(out=xsB[:], in_=B1b[:, 0:768]), r=['B1'], w=['xsB'])
                        xs3 = xsB[:, 0:512].rearrange("p (r d) -> p r d", r=8)
                        S.pool(lambda e: e.tensor_tensor(out=xdt[:].rearrange("p (r d) -> p r d", r=8), in0=xs3,
                                                         in1=dt_t[:].unsqueeze(2).to_broadcast([128, 8, 64]), op=ALU.mult),
                               r=['xsB', K_('dt_t')], w=['xdt'])
                        S.pool(lambda e: e.tensor_tensor(out=xsD[:].rearrange("p (r d) -> p r d", r=8), in0=xs3,
                                                         in1=pb[:, P_DSK:P_DSK + 8].unsqueeze(2).to_broadcast([128, 8, 64]), op=ALU.mult),
                               r=['xsB', 'pb'], w=['xsD'])
                        S.pool(lambda e: e.tensor_tensor(out=xdtd[:].rearrange("p (r d) -> p r d", r=8),
                                                         in0=xdt[:].rearrange("p (r d) -> p r d", r=8),
                                                         in1=dst[:].unsqueeze(2).to_broadcast([128, 8, 64]), op=ALU.mult),
                               r=['xdt', K_('dst')], w=['xdtd'])
                        yield
                        for g in range(2):
                            pcb = B[4][:, 352:480]
                            S.pe(lambda e: e.matmul(pcb, xbcT[:, 4 + g, tk], xbcT[:, 6 + g, tk], start=True, stop=True),
                                 r=['xbcT'], w=['B4'])
                            S.pe(lambda e: e.matmul(B[5][:, :], ones[0:96, :], BDacs[0:96, g * 512:(g + 1) * 512], start=True, stop=False),
                                 r=['cst', K_('BDacs')], w=['B5'])
                            S.pe(lambda e: e.matmul(B[5][:, :], nacsT[0:96, :], bmask96[:, g * 512:(g + 1) * 512], start=False, stop=False),
                                 r=['cst', K_('nacsT')], w=['B5'])
                            S.pe(lambda e: e.matmul(B[5][:, :], ident[:], negm[:], start=False, stop=True),
                                 r=['ident', 'negm'], w=['B5'])
                            S.act(lambda e: e.activation(out=dec[g][:], in_=B[5][:, :], func=AF.Exp), r=['B5'], w=['dec'])
                            S.dve(lambda e: e.tensor_tensor(
                                out=MT[g][:].rearrange("p (r l) -> p r l", r=4),
                                in0=pcb.unsqueeze(1).to_broadcast([128, 4, 128]),
                                in1=dec[g][:].rearrange("p (r l) -> p r l", r=4), op=ALU.mult),
                                r=['B4', 'dec'], w=['MT%d' % g])
                            yield
                        for g in range(2):
                            S.pe(lambda e, g=g: e.matmul(B[5][:, g * 256:(g + 1) * 256], xbcT[:, 6 + g, tk], Sbf[:, g * 256:(g + 1) * 256],
                                                         start=True, stop=True), r=['xbcT', 'Sbf'], w=['B5'])
                        S.dve(lambda e: e.tensor_tensor(out=y1[:].rearrange("p (r d) -> p r d", r=8),
                                                        in0=B[5][:, :].rearrange("p (r d) -> p r d", r=8),
                                                        in1=ea[:].unsqueeze(2).to_broadcast([128, 8, 64]), op=ALU.mult),
                              r=['B5', K_('ea')], w=['y1'])
                        yield
                        for r_ in range(8):
                            g = r_ // 4
                            S.pe(lambda e, r_=r_, g=g: e.matmul(B[5][:, r_ * 64:(r_ + 1) * 64], MT[g][:, (r_ % 4) * 128:(r_ % 4 + 1) * 128],
                                                                xdt[:, r_ * 64:(r_ + 1) * 64], start=(r_ == 0), stop=False, skip_group_check=True),
                                 r=['MT%d' % g, 'xdt'], w=['B5'])
                        S.pe(lambda e: e.matmul(B[5][:, :], ident[:], xsD[:], start=False, stop=True, skip_group_check=True),
                             r=['ident', 'xsD'], w=['B5'])
                        S.dve(lambda e: e.tensor_tensor(out=y1[:], in0=B[5][:, :], in1=y1[:], op=ALU.add), r=['B5', 'y1'], w=['y1'])
                        S.dve(lambda e: e.tensor_tensor(out=gy[:], in0=y1[:], in1=zs[tt][:], op=ALU.mult),
                              r=['y1', 'zs%d' % tt], w=['gy'])
                        S.act(lambda e: e.activation(out=yn[:], in_=gy[:], func=AF.Square, accum_out=ssq2[:]),
                              r=['gy'], w=['yn', 'ssq2'])
                        S.act(lambda e: e.activation(out=rstd2[:], in_=ssq2[:], func=AF.Sqrt, scale=1.0 / 512, bias=EPS),
                              r=['ssq2'], w=['rstd2'])
                        S.dve(lambda e: e.reciprocal(out=rstd2[:], in_=rstd2[:]), r=['rstd2'], w=['rstd2'])
                        S.dve(lambda e: e.scalar_tensor_tensor(out=yn[:], in0=gy[:], scalar=rstd2[:, 0:1], in1=pb[:, P_GSSD:P_GSSD + 512],
                                                               op0=ALU.mult, op1=ALU.mult), r=['gy', 'rstd2', 'pb'], w=['yn'])
                        yield
                        for g in range(2):
                            S.pe(lambda e, g=g: e.matmul(B[5][:, g * 256:(g + 1) * 256], xsB[:, 512 + g * 128:512 + (g + 1) * 128],
                                                         xdtd[:, g * 256:(g + 1) * 256], start=True, stop=True),
                                 r=['xsB', 'xdtd'], w=['B5'])
                        S.dve(lambda e: e.tensor_tensor(out=S32[:].rearrange("p (r d) -> p r d", r=8),
                                                        in0=S32[:].rearrange("p (r d) -> p r d", r=8),
                                                        in1=cd_b[:].unsqueeze(2).to_broadcast([128, 8, 64]), op=ALU.mult),
                              r=['S32', K_('cd_b')], w=['S32'])
                        S.dve(lambda e: e.tensor_tensor(out=S32[:], in0=B[5][:, :], in1=S32[:], op=ALU.add),
                              r=['B5', 'S32'], w=['S32'])
                        S.pool(lambda e: e.tensor_copy(out=Sbf[:], in_=S32[:]), r=['S32'], w=['Sbf'])
                        yield
                        for c in range(4):
                            S.pe(lambda e, c=c: e.transpose(B1b[:, c * 128:(c + 1) * 128], yn[:, c * 128:(c + 1) * 128], ident[:]),
                                 r=['yn', 'ident'], w=['B1'])
                        S.dve(lambda e: e.tensor_copy(out=mixT[:, :, tk], in_=B1b[:, 0:512].rearrange("p (c l) -> p c l", c=4)),
                              r=['B1'], w=['mixT'])
                        yield

                def attn_gen():
                    nkt = 2 * b + 2
                    tiles = [(h, kt) for h in range(8) for kt in range(nkt)]

                    def qk(i):
                        h, kt = tiles[i]
                        dg = kt - 2 * b
                        c0 = 0 if dg < 0 else dg * 128
                        bsel = (2, 7, 0)[i % 3] if OPT['la'] == 2 else (2, 7)[i % 2]
                        pS = B[bsel][:, 0:TB]; kps = 'B%d' % bsel
                        ks_ = slice(kt * 128, (kt + 1) * 128)
                        if dg < 0:
                            S.pe(lambda e: e.matmul(pS[:, 0:TB], KT[h][0:96, ks_], QT[h][0:96, 0:TB], start=True, stop=True),
                                 r=['KT%d' % h, 'QT%d' % h], w=[kps])
                        else:
                            S.pe(lambda e: e.matmul(pS[:, c0:c0 + 128], ident[:], negm[:, 0:128], start=True, stop=False),
                                 r=['ident', 'negm'], w=[kps])
                            S.pe(lambda e: e.matmul(pS[:, c0:c0 + 128], KT[h][0:96, ks_], QT[h][0:96, c0:c0 + 128], start=False, stop=True),
                                 r=['KT%d' % h, 'QT%d' % h], w=[kps])
                            if c0 + 128 < TB:
                                S.pe(lambda e: e.matmul(pS[:, c0 + 128:TB], KT[h][0:96, ks_], QT[h][0:96, c0 + 128:TB], start=True, stop=True),
                                     r=['KT%d' % h, 'QT%d' % h], w=[kps])
                        P_ = PT[i % 3]; kpt = 'PT%d' % (i % 3)
                        S.act(lambda e: e.activation(out=P_[:, c0:TB], in_=pS[:, c0:TB], func=AF.Exp, bias=Fk[:, kt, h:h + 1]),
                              r=[kps, 'Fk'], w=[kpt])

                    def pv(i):
                        h, kt = tiles[i]
                        dg = kt - 2 * b
                        c0 = 0 if dg < 0 else dg * 128
                        P_ = PT[i % 3]; kpt = 'PT%d' % (i % 3)
                        ob = (3, 6)[h % 2]
                        pO = B[ob][0:65, 0:TB]
                        S.pe(lambda e: e.matmul(pO[:, c0:TB], Vx[:, kt, h, :], P_[:, c0:TB], start=(kt == 0), stop=(kt == nkt - 1),
                                                skip_group_check=True), r=['Vx', kpt], w=['B%d' % ob])
                        if kt == nkt - 1:
                            S.act(lambda e: e.activation(out=Osb8all[:, (h % 4) * TB:(h % 4 + 1) * TB], in_=pO, func=AF.Copy),
                                  r=['B%d' % ob], w=['Osb8'])
                            if h % 4 == 3:
                                S.dve(lambda e: e.reciprocal(out=Osb8all[64:65, :], in_=Osb8all[64:65, :]), r=['Osb8'], w=['Osb8'])
                                pend.append([h - 3, i + min(12, nkt - 1)])

                    pend = []

                    def normb(h0):
                        for pr in range(2):
                            cs2 = slice(pr * 2 * TB, (pr + 1) * 2 * TB)
                            S.pe(lambda e: e.matmul(B[0][0:64, :], cst[64:65, C_ONE - 128:C_ONE - 64], Osb8all[64:65, cs2], start=True, stop=True),
                                 r=['cst', 'Osb8'], w=['B0'])
                            S.dve(lambda e: e.tensor_tensor(out=yTall[0:64, h0 + 2 * pr:h0 + 2 * pr + 2, :],
                                                            in0=Osb8all[0:64, cs2].rearrange("p (a b) -> p a b", a=2),
                                                            in1=B[0][0:64, :].rearrange("p (a b) -> p a b", a=2), op=ALU.mult),
                                  r=['Osb8', 'B0'], w=['yT'])

                    if parts & 4:
                        LA = OPT['la']
                        for i0_ in range(min(LA, len(tiles))):
                            qk(i0_)
                        for i in range(len(tiles)):
                            if i + LA < len(tiles):
                                qk(i + LA)
                            pv(i)
                            if pend and i >= pend[0][1] and i + 1 < len(tiles):
                                normb(pend.pop(0)[0])
                            yield
                        yield 'tail'
                        normb(pend.pop(0)[0])
                        yield

                gs = ssd_gen() if parts & 2 else iter(())
                ga = attn_gen()
                n_attn = 8 * (2 * b + 2)
                n_ssd = 14
                every = max(1, n_attn // (2 * (n_ssd + 1)))
                ia = 0
                pre_done = False
                ssd_done = False; attn_done = False
                pe_done = False
                while not (ssd_done and attn_done):
                    if not attn_done:
                        try:
                            tag = next(ga); ia += 1
                            if tag == 'tail' and b + 1 < nb and OPT['prefA']:
                                if not pre_done:
                                    secA_pre(b + 1); pre_done = True
                                secA_pe(b + 1); pe_done = True
                        except StopIteration:
                            attn_done = True
                    if OPT['prefA'] and not pre_done and b + 1 < nb and (attn_done or ia >= n_attn // 2):
                        secA_pre(b + 1)
                        pre_done = True
                    if not ssd_done and (attn_done or ia % every == 0):
                        try:
                            next(gs)
                        except StopIteration:
                            ssd_done = True
                if b + 1 < nb and OPT['prefA'] and not pe_done:
                    if not pre_done:
                        secA_pre(b + 1)
                    secA_pe(b + 1)

                wo_s, kws = wview(ci0 + 6)
                wo_f0, kwf0 = wview(ci0 + 7)
                wo_f1, kwf1 = wview(ci0 + 8)
                wo_f0 = wbuf[(ci0 + 7) % 3][0:96, :].rearrange("p (a b) -> p a b", a=4)
                wo_f1 = wbuf[(ci0 + 8) % 3][0:96, :].rearrange("p (a b) -> p a b", a=4)
                combos = [(tt, hf) for tt in range(2) for hf in range(2)]
                fbank = (2, 3, 6, 7)
                for ci_, (tt, hf) in enumerate(combos):
                    po = B[fbank[ci_]]; kp = 'B%d' % fbank[ci_]
                    tk = slice(tt * 128, (tt + 1) * 128); cs = slice(hf * 512, (hf + 1) * 512)
                    for c in range(4):
                        S.pe(lambda e, c=c: e.matmul(po[:, :], mixT[:, c, tk], wo_s[:, c, cs], start=(c == 0), stop=False),
                             r=['mixT', kws], w=[kp])
                wload()
                for (wf, kwf, hh0) in ((wo_f0, kwf0, 0), (wo_f1, kwf1, 4)):
                    for ci_, (tt, hf) in enumerate(combos):
                        po = B[fbank[ci_]]; kp = 'B%d' % fbank[ci_]
                        tk = slice(tt * 128, (tt + 1) * 128); cs = slice(hf * 512, (hf + 1) * 512)
                        for h in range(hh0, hh0 + 4):
                            S.pe(lambda e, h=h: e.matmul(po[:, :], yTall[0:96, h, tk], wf[0:96, h % 4, cs], start=False, stop=(h == 7)),
                                 r=['yT', kwf], w=[kp])
                    wload()
                for ci_, (tt, hf) in enumerate(combos):
                    po = B[fbank[ci_]]; kp = 'B%d' % fbank[ci_]
                    cs = slice(hf * 512, (hf + 1) * 512)
                    X = xr[tt]; kx = 'xr%d' % tt
                    S.dve(lambda e: e.tensor_tensor(out=X[:, cs], in0=po[:, :], in1=X[:, cs], op=ALU.add), r=[kp, kx], w=[kx])
                for tt in range(2):
                    r0 = t0 + tt * 128
                    S.dma('sp', h1d[r0:r0 + 128, :], xr[tt][:], r=['xr%d' % tt], w=['h1d'])

            S.emit()
        if stage == 1:
            with ExitStack() as es:
                for i in range(nb):
                    S.dma('sp', out[i * 256:(i + 1) * 256, :], h1d[i * 256:(i + 1) * 256, :], r=['h1d'], w=['out'])
                S.emit()
            return nc
        phase2(nc, S, h1d, p_in, w_rt, w_g, w_u, w_d, w_pg, w_pp, pbd, cstd, out, nb2, nexp)
    return nc


def phase2(nc, S, h1d, p_in, w_rt, w_g, w_u, w_d, w_pg, w_pp, pbd, cstd, out, nb2, nexp):
    NT = T2 // 128
    AXX = mybir.AxisListType.X
    with ExitStack() as es:
        def sb(name, shape, dt=F32):
            return es.enter_context(nc.sbuf_tensor("u_" + name, shape, dt))

        B = [es.enter_context(nc.psum_tensor("C%d" % i, [128, 512], F32)) for i in range(8)]
        B0b = B[0][:].bitcast(BF16)
        identf = sb("identf", [128, 128]); pb = sb("pb", [128, P_W])
        ident = sb("ident", [128, 128], BF16)
        wpg = sb("wpg", [128, 8, 1024], BF16); wpp = sb("wpp", [128, 2, 1024], BF16)
        wrt = sb("wrt", [128, 8, 20], BF16)
        eg = [sb("eg%d" % i, [128, 8, 512], BF16) for i in range(2)]
        eu = [sb("eu%d" % i, [128, 8, 512], BF16) for i in range(2)]
        ed = [sb("ed%d" % i, [128, 4, 1024], BF16) for i in range(2)]
        acc = sb("acc", [128, NT, 1024])
        hn2T = [sb("hn2T%d" % i, [128, 8, T2], BF16) for i in range(2)]
        cw = [sb("cw%d" % i, [128, NT, 16]) for i in range(2)]
        hidT = [sb("hidT%d" % i, [128, 4, 512], BF16) for i in range(2)]
        sg = [sb("sg%d" % i, [128, 512]) for i in range(2)]
        h1t = [sb("h1t%d" % i, [128, 1024]) for i in range(2)]
        xn = [sb("xn%d" % i, [128, 1024], BF16) for i in range(2)]
        junk = sb("junk", [128, 1024], BF16); junkF = sb("junkF", [128, 1024], BF16)
        ssqF = [sb("ssqF%d" % i, [128, 1]) for i in range(2)]; rstdF = [sb("rstdF%d" % i, [128, 1]) for i in range(2)]
        ssq = [sb("ssq%d" % i, [128, 1]) for i in range(2)]; rstd = [sb("rstd%d" % i, [128, 1]) for i in range(2)]
        lg = sb("lg", [128, NT, 20])
        gmax = sb("gmax", [128, NT]); oh = sb("oh", [128, NT, 4]); d4 = sb("d4", [128, NT, 4]); gsum = sb("gsum", [128, NT])
        pgr = sb("pgr", [128, NT]); ing = sb("ing", [128, NT, 4]); t4 = sb("t4", [128, NT, 4]); m1 = sb("m1", [128, NT])
        mk1 = sb("mk1", [128, NT, 4]); in2 = sb("in2", [128, NT, 4]); m2 = sb("m2", [128, NT]); mk2 = sb("mk2", [128, NT, 4])
        dm = sb("dm", [128, NT]); w1 = sb("w1", [128, NT]); w1p = sb("w1p", [128, NT]); w2p = sb("w2p", [128, NT])
        wexp = sb("wexp", [128, NT, 4])
        hn3T = [sb("hn3T%d" % i, [128, 8, 128], BF16) for i in range(2)]
        pt = [sb("pt%d" % i, [128, 256]) for i in range(2)]; pbf = [sb("pbf%d" % i, [128, 256], BF16) for i in range(2)]
        pT = [sb("pT%d" % i, [128, 2, 128], BF16) for i in range(2)]
        gsb = [sb("gsb%d" % i, [128, 512]) for i in range(2)]
        tmp = [sb("tmp%d" % i, [128, 512]) for i in range(2)]
        ot = [sb("ot%d" % i, [128, 1024]) for i in range(2)]

        S.dma('sp', identf[:], cstd[:, C_ID:C_ID + 128], w=['identf'])
        S.dma('sp', pb[:], pbd[:, :], w=['pb'])
        S.dve(lambda e: e.tensor_copy(out=ident[:], in_=identf[:]), r=['identf'], w=['ident'])
        S.dma('pool', wrt[:], w_rt.rearrange("(kc p) c -> p kc c", p=128), w=['wrt'])

        est = {'n': 0}
        total_e = nb2 * nexp

        def eload():
            n = est['n']
            if n >= total_e:
                return
            est['n'] += 1
            e_ = n % nexp; bi = n % 2
            S.dma('pool', eg[bi][:], w_g[e_].rearrange("(kc p) f -> p kc f", p=128), w=['eg%d' % bi])
            S.dma('pool', eu[bi][:], w_u[e_].rearrange("(kc p) f -> p kc f", p=128), w=['eu%d' % bi])
            S.dma('pool', ed[bi][:], w_d[e_].rearrange("(kc p) f -> p kc f", p=128), w=['ed%d' % bi])

        eload(); eload()
        S.dma('pool', wpg[:], w_pg.rearrange("(kc p) c -> p kc c", p=128), w=['wpg'])
        S.dma('pool', wpp[:], w_pp.rearrange("(kc p) c -> p kc c", p=128), w=['wpp'])

        def norm_pre(src, kx, j):
            S.act(lambda e: e.activation(out=junk[:], in_=src, func=AF.Square, accum_out=ssq[j][:]), r=[kx], w=['junk', 'ssq%d' % j])
            S.act(lambda e: e.activation(out=rstd[j][:], in_=ssq[j][:], func=AF.Sqrt, scale=1.0 / D, bias=EPS), r=['ssq%d' % j], w=['rstd%d' % j])
            S.dve(lambda e: e.reciprocal(out=rstd[j][:], in_=rstd[j][:]), r=['rstd%d' % j], w=['rstd%d' % j])
            S.pool(lambda e: e.tensor_scalar(out=xn[j][:], in0=src, scalar1=rstd[j][:, 0:1], scalar2=1.0, op0=ALU.mult, op1=ALU.mult),
                   r=[kx, 'rstd%d' % j], w=['xn%d' % j])

        def norm_pe(j):
            for kc in range(8):
                S.pe(lambda e, kc=kc: e.transpose(B0b[:, kc * 128:(kc + 1) * 128], xn[j][:, kc * 128:(kc + 1) * 128], ident[:]),
                     r=['xn%d' % j, 'ident'], w=['C0'])

        def prologue_gen(tb):
            T0 = tb * T2
            HT = hn2T[tb % 2]; kht = 'hn2T%d' % (tb % 2)
            CW = cw[tb % 2]; kcw = 'cw%d' % (tb % 2)
            def pro_pre(i):
                r0 = T0 + i * 128
                j = i % 2
                S.dma('sp', h1t[j][:], h1d[r0:r0 + 128, :], r=['h1d'], w=['h1t%d' % j])
                norm_pre(h1t[j][:], 'h1t%d' % j, j)

            pro_pre(0)
            for i in range(NT):
                j = i % 2
                if i + 1 < NT:
                    pro_pre(i + 1)
                norm_pe(j)
                S.dve(lambda e: e.tensor_tensor(out=HT[:, :, i * 128:(i + 1) * 128], in0=B0b.rearrange("p (a b) -> p a b", a=8),
                                                in1=pb[:, P_GFFN:P_GFFN + 8].unsqueeze(2).to_broadcast([128, 8, 128]), op=ALU.mult),
                      r=['C0', 'pb'], w=[kht])
                yield
                for kc in range(8):
                    S.pe(lambda e, kc=kc: e.matmul(B[1][:, 0:20], HT[:, kc, i * 128:(i + 1) * 128], wrt[:, kc, :],
                                                   start=(kc == 0), stop=(kc == 7)), r=[kht, 'wrt'], w=['C1'])
                S.dve(lambda e: e.tensor_tensor(out=lg[:, i, :], in0=B[1][:, 0:20], in1=pb[:, P_BR:P_BR + 20], op=ALU.add),
                      r=['C1', 'pb'], w=['lg'])
                yield
            bc4 = lambda a: a[:].unsqueeze(2).to_broadcast([128, NT, 4])
            S.dve(lambda e: e.tensor_reduce(out=gmax[:], in_=lg[:, :, 0:4], axis=AXX, op=ALU.max), r=['lg'], w=['gmax'])
            S.dve(lambda e: e.tensor_tensor(out=oh[:], in0=lg[:, :, 0:4], in1=bc4(gmax), op=ALU.is_equal), r=['lg', 'gmax'], w=['oh'])
            S.dve(lambda e: e.tensor_tensor(out=d4[:], in0=lg[:, :, 0:4], in1=bc4(gmax), op=ALU.subtract), r=['lg', 'gmax'], w=['d4'])
            S.act(lambda e: e.activation(out=d4[:], in_=d4[:], func=AF.Exp), r=['d4'], w=['d4'])
            S.dve(lambda e: e.tensor_reduce(out=gsum[:], in_=d4[:], axis=AXX, op=ALU.add), r=['d4'], w=['gsum'])
            S.dve(lambda e: e.reciprocal(out=pgr[:], in_=gsum[:]), r=['gsum'], w=['pgr'])
            S.dve(lambda e: e.tensor_tensor(out=ing[:], in0=lg[:, :, 4:8], in1=oh[:, :, 0:1].to_broadcast([128, NT, 4]), op=ALU.mult),
                  r=['lg', 'oh'], w=['ing'])
            for g in range(1, 4):
                S.dve(lambda e, g=g: e.tensor_tensor(out=t4[:], in0=lg[:, :, 4 + 4 * g:8 + 4 * g], in1=oh[:, :, g:g + 1].to_broadcast([128, NT, 4]),
                                                     op=ALU.mult), r=['lg', 'oh'], w=['t4'])
                S.dve(lambda e: e.tensor_tensor(out=ing[:], in0=ing[:], in1=t4[:], op=ALU.add), r=['ing', 't4'], w=['ing'])
            yield
            S.dve(lambda e: e.tensor_reduce(out=m1[:], in_=ing[:], axis=AXX, op=ALU.max), r=['ing'], w=['m1'])
            S.dve(lambda e: e.tensor_tensor(out=mk1[:], in0=ing[:], in1=bc4(m1), op=ALU.is_equal), r=['ing', 'm1'], w=['mk1'])
            S.dve(lambda e: e.scalar_tensor_tensor(out=in2[:], in0=mk1[:], scalar=NEG, in1=ing[:], op0=ALU.mult, op1=ALU.add),
                  r=['mk1', 'ing'], w=['in2'])
            S.dve(lambda e: e.tensor_reduce(out=m2[:], in_=in2[:], axis=AXX, op=ALU.max), r=['in2'], w=['m2'])
            S.dve(lambda e: e.tensor_tensor(out=mk2[:], in0=in2[:], in1=bc4(m2), op=ALU.is_equal), r=['in2', 'm2'], w=['mk2'])
            S.dve(lambda e: e.tensor_tensor(out=dm[:], in0=m2[:], in1=m1[:], op=ALU.subtract), r=['m1', 'm2'], w=['dm'])
            S.act(lambda e: e.activation(out=w1[:], in_=dm[:], func=AF.Exp), r=['dm'], w=['w1'])
            S.dve(lambda e: e.tensor_scalar(out=w1[:], in0=w1[:], scalar1=1.0, scalar2=None, op0=ALU.add), r=['w1'], w=['w1'])
            S.dve(lambda e: e.reciprocal(out=w1[:], in_=w1[:]), r=['w1'], w=['w1'])
            S.dve(lambda e: e.tensor_tensor(out=w1p[:], in0=w1[:], in1=pgr[:], op=ALU.mult), r=['w1', 'pgr'], w=['w1p'])
            S.dve(lambda e: e.tensor_tensor(out=w2p[:], in0=pgr[:], in1=w1p[:], op=ALU.subtract), r=['w1p', 'pgr'], w=['w2p'])
            yield
            S.dve(lambda e: e.tensor_tensor(out=wexp[:], in0=mk1[:], in1=bc4(w1p), op=ALU.mult), r=['mk1', 'w1p'], w=['wexp'])
            S.dve(lambda e: e.tensor_tensor(out=t4[:], in0=mk2[:], in1=bc4(w2p), op=ALU.mult), r=['mk2', 'w2p'], w=['t4'])
            S.dve(lambda e: e.tensor_tensor(out=wexp[:], in0=wexp[:], in1=t4[:], op=ALU.add), r=['wexp', 't4'], w=['wexp'])
            for g in range(4):
                S.dve(lambda e, g=g: e.tensor_tensor(out=CW[:, :, 4 * g:4 * g + 4], in0=wexp[:], in1=oh[:, :, g:g + 1].to_broadcast([128, NT, 4]),
                                                     op=ALU.mult), r=['wexp', 'oh'], w=[kcw])
            yield

        uic = [0]

        def gu(tb, e_, sub):
            HT = hn2T[tb % 2]; kht = 'hn2T%d' % (tb % 2)
            bi = (tb * nexp + e_) % 2
            G = eg[bi]; U = eu[bi]
            ts_ = slice(sub * 512, (sub + 1) * 512)
            H = hidT[sub]; kh = 'hidT%d' % sub
            for fcn in range(4):
                ui = uic[0]; uic[0] += 1
                pg = B[2 + ui % 2]; kpg = 'C%d' % (2 + ui % 2)
                pu = B[4 + ui % 2]; kpu = 'C%d' % (4 + ui % 2)
                SG = sg[ui % 2]; ksg = 'sg%d' % (ui % 2)
                for kc in range(8):
                    S.pe(lambda e, kc=kc: e.matmul(pg[:, :], G[:, kc, fcn * 128:(fcn + 1) * 128], HT[:, kc, ts_],
                                                   start=(kc == 0), stop=(kc == 7)), r=['eg%d' % bi, kht], w=[kpg])
                for kc in range(8):
                    S.pe(lambda e, kc=kc: e.matmul(pu[:, :], U[:, kc, fcn * 128:(fcn + 1) * 128], HT[:, kc, ts_],
                                                   start=(kc == 0), stop=(kc == 7)), r=['eu%d' % bi, kht], w=[kpu])
                S.act(lambda e: e.activation(out=SG[:], in_=pg[:, :], func=AF.Silu), r=[kpg], w=[ksg])
                S.dve(lambda e: e.tensor_tensor(out=H[:, fcn, :], in0=pu[:, :], in1=SG[:], op=ALU.mult), r=[kpu, ksg], w=[kh])

        def dn(tb, e_, sub):
            CW = cw[tb % 2]; kcw = 'cw%d' % (tb % 2)
            bi = (tb * nexp + e_) % 2
            Dn = ed[bi]
            H = hidT[sub]; kh = 'hidT%d' % sub
            for j in range(4):
                ti = sub * 4 + j
                for hf in range(2):
                    pd = B[6 + hf]; kpd = 'C%d' % (6 + hf)
                    cs = slice(hf * 512, (hf + 1) * 512)
                    for fcn in range(4):
                        S.pe(lambda e, fcn=fcn: e.matmul(pd[:, :], H[:, fcn, j * 128:(j + 1) * 128], Dn[:, fcn, cs],
                                                         start=(fcn == 0), stop=(fcn == 3)), r=[kh, 'ed%d' % bi], w=[kpd])
                    S.dve(lambda e: e.scalar_tensor_tensor(out=acc[:, ti, cs], in0=pd[:, :], scalar=CW[:, ti, e_:e_ + 1], in1=acc[:, ti, cs],
                                                           op0=ALU.mult, op1=ALU.add), r=[kpd, kcw, 'acc%d' % ti], w=['acc%d' % ti])

        NSUB = T2 // 512

        def experts_gen(tb, pre):
            for e_ in range(nexp):
                if e_ == 0 and pre:
                    for sub in range(NSUB):
                        dn(tb, e_, sub)
                        yield
                else:
                    for sub in range(NSUB):
                        gu(tb, e_, sub)
                        yield
                        dn(tb, e_, sub)
                        yield
                eload()

        def epilogue_gen(tb):
            T0 = tb * T2

            def epi_pre(i):
                r0 = T0 + i * 128
                j = i % 2
                norm_pre(acc[:, i, :], 'acc%d' % i, j)
                S.dma('sp', pt[j][:], p_in[r0:r0 + 128, :], w=['pt%d' % j])
                S.pool(lambda e: e.tensor_copy(out=pbf[j][:], in_=pt[j][:]), r=['pt%d' % j], w=['pbf%d' % j])

            epi_pre(0)
            for i in range(NT):
                r0 = T0 + i * 128
                ka = 'acc%d' % i
                j = i % 2
                norm_pe(j)
                S.dve(lambda e: e.tensor_tensor(out=hn3T[j][:], in0=B0b.rearrange("p (a b) -> p a b", a=8),
                                                in1=pb[:, P_GPLE:P_GPLE + 8].unsqueeze(2).to_broadcast([128, 8, 128]), op=ALU.mult),
                      r=['C0', 'pb'], w=['hn3T%d' % j])
                for c in range(2):
                    S.pe(lambda e, c=c: e.transpose(B[1][:].bitcast(BF16)[:, c * 128:(c + 1) * 128], pbf[j][:, c * 128:(c + 1) * 128], ident[:]),
                         r=['pbf%d' % j, 'ident'], w=['C1'])
                S.dve(lambda e: e.tensor_copy(out=pT[j][:], in_=B[1][:].bitcast(BF16)[:, 0:256].rearrange("p (a b) -> p a b", a=2)),
                      r=['C1'], w=['pT%d' % j])
                if i + 1 < NT:
                    epi_pre(i + 1)
                yield
                for hf in range(2):
                    cs = slice(hf * 512, (hf + 1) * 512)
                    pga = B[6]; kga = 'C6'
                    ppp = B[7]; kpp = 'C7'
                    for kc in range(8):
                        S.pe(lambda e, kc=kc: e.matmul(pga[:, :], hn3T[j][:, kc, :], wpg[:, kc, cs], start=(kc == 0), stop=(kc == 7)),
                             r=['hn3T%d' % j, 'wpg'], w=[kga])
                    for kc in range(2):
                        S.pe(lambda e, kc=kc: e.matmul(ppp[:, :], pT[j][:, kc, :], wpp[:, kc, cs], start=(kc == 0), stop=(kc == 1)),
                             r=['pT%d' % j, 'wpp'], w=[kpp])
                    S.act(lambda e: e.activation(out=gsb[hf][:], in_=pga[:, :], func=AF.Sigmoid), r=[kga], w=['gsb%d' % hf])
                    S.dve(lambda e: e.tensor_tensor(out=tmp[hf][:], in0=ppp[:, :], in1=gsb[hf][:], op=ALU.mult), r=[kpp, 'gsb%d' % hf], w=['tmp%d' % hf])
                    S.dve(lambda e: e.tensor_tensor(out=acc[:, i, cs], in0=tmp[hf][:], in1=acc[:, i, cs], op=ALU.add), r=['tmp%d' % hf, ka], w=[ka])
                S.act(lambda e: e.activation(out=junkF[:], in_=acc[:, i, :], func=AF.Square, accum_out=ssqF[j][:]), r=[ka], w=['junkF', 'ssqF%d' % j])
                S.act(lambda e: e.activation(out=rstdF[j][:], in_=ssqF[j][:], func=AF.Sqrt, scale=1.0 / D, bias=EPS), r=['ssqF%d' % j], w=['rstdF%d' % j])
                S.dve(lambda e: e.reciprocal(out=rstdF[j][:], in_=rstdF[j][:]), r=['rstdF%d' % j], w=['rstdF%d' % j])
                O = ot[j]; ko = 'ot%d' % j
                S.dve(lambda e: e.scalar_tensor_tensor(out=O[:], in0=acc[:, i, :], scalar=rstdF[j][:, 0:1], in1=pb[:, P_GFIN:P_GFIN + 1024],
                                                       op0=ALU.mult, op1=ALU.mult), r=[ka, 'rstdF%d' % j, 'pb'], w=[ko])
                S.dma('sp', out[r0:r0 + 128, :], O[:], r=[ko], w=['out'])
                if tb + 1 < nb2:
                    r1 = (tb + 1) * T2 + i * 128
                    S.dma('sp', acc[:, i, :], h1d[r1:r1 + 128, :], r=['h1d', ka], w=[ka])
                yield

        def load_acc(tb):
            for i in range(NT):
                r0 = tb * T2 + i * 128
                S.dma('sp', acc[:, i, :], h1d[r0:r0 + 128, :], r=['h1d'], w=['acc%d' % i])

        for _ in prologue_gen(0):
            pass
        load_acc(0)
        pre = False
        for tb in range(nb2):
            ge = experts_gen(tb, pre)
            gp = prologue_gen(tb + 1) if tb + 1 < nb2 else iter(())
            n_e = nexp * NSUB * 2
            n_p = 2 * NT + 3
            start_at = 4
            every = max(1, (n_e - start_at - 2) // (n_p + 1))
            k = 0
            for _ in ge:
                k += 1
                if k >= start_at and (k - start_at) % every == 0:
                    next(gp, None)
            for _ in gp:
                pass
            gepi = epilogue_gen(tb)
            if tb + 1 < nb2:
                steps = [(tb + 1, 0, sub) for sub in range(NSUB)]
                per = max(1, (2 * NT) // (len(steps) + 1))
                kk = 0
                for _ in gepi:
                    kk += 1
                    if steps and kk % per == 0:
                        gu(*steps.pop(0))
                for st in steps:
                    gu(*st)
                pre = True
            else:
                for _ in gepi:
                    pass
        S.emit()


def _consts():
    c = np.zeros((128, C_W), np.float32)
    c[:, C_ID:C_ID + 128] = np.eye(128, dtype=np.float32)
    j = np.arange(128)
    c[:, C_TRI:C_TRI + 128] = (j[:, None] <= j[None, :]).astype(np.float32)
    c[:, C_ONE:C_ONE + 128] = 1.0
    c[:, C_NEG:C_NEG + 128] = np.where(j[None, :] < j[:, None], NEG, 0.0).astype(np.float32)
    bm = np.zeros((8, 8, 128), np.float32)
    for k in range(8):
        bm[k, k, :] = 1.0
    c[0:8, C_BM:C_BM + 1024] = bm.reshape(8, 1024)
    return c


def _prep(inp):
    f = lambda a: np.ascontiguousarray(np.asarray(a, dtype=np.float32))
    L = 0
    pb = np.zeros((128, P_W), np.float32)
    bc = lambda v: np.broadcast_to(f(v).reshape(1, -1), (128, f(v).size))
    pb[:, P_FB:P_FB + 8] = bc(inp['fox_fbias'][L])
    pb[:, P_DTB:P_DTB + 8] = bc(inp['dt_bias'][L])
    pb[:, P_ALOG:P_ALOG + 8] = bc(inp['a_log'][L])
    pb[:, P_DSK:P_DSK + 8] = bc(inp['d_skip'][L])
    pb[:, P_GMIX:P_GMIX + 8] = f(inp['g_mix'][L]).reshape(8, 128).T
    pb[:, P_GFFN:P_GFFN + 8] = f(inp['g_ffn'][L]).reshape(8, 128).T
    pb[:, P_GPLE:P_GPLE + 8] = f(inp['g_ple'][L]).reshape(8, 128).T
    pb[:, P_CB:P_CB + 8] = f(inp['conv_b'][L]).reshape(8, 128).T
    cw = f(inp['conv_w'][L]).reshape(4, 8, 128)
    pb[:, P_CW:P_CW + 32] = cw.transpose(2, 1, 0).reshape(128, 32)
    pb[:, P_BR:P_BR + 4] = bc(inp['b_route_group'][L])
    pb[:, P_BR + 4:P_BR + 20] = bc(inp['b_route_expert'][L])
    pb[:, P_GSSD:P_GSSD + 512] = bc(inp['g_ssd'][L])
    pb[:, P_GFIN:P_GFIN + 1024] = bc(inp['g_final'])
    w_in = f(inp['w_in'][L])
    shared = {
        'w_in': w_in,
        'w_sm': np.ascontiguousarray(np.concatenate([w_in[:, 3080:3088], w_in[:, 1536:1544]], axis=1)),
        'w_out': f(inp['w_out'][L]),
        'w_rt': np.ascontiguousarray(np.concatenate([f(inp['w_route_group'][L]), f(inp['w_route_expert'][L])], axis=1)),
        'w_g': f(inp['w_exp_gate'][L]).reshape(16, D, 512),
        'w_u': f(inp['w_exp_up'][L]).reshape(16, D, 512),
        'w_d': f(inp['w_exp_down'][L]).reshape(16, 512, D),
        'w_pg': f(inp['w_ple_gate'][L]),
        'w_pp': f(inp['w_ple_proj'][L]),
        'pb': pb,
        'cst': _consts(),
    }
    x = f(inp['x']); p = f(inp['p'][L])
    return [dict(shared, x=x[b], p=p[b]) for b in range(8)]


def kernel(**inputs):
    stage = _CACHE.get('stage', 3)
    nc = build(stage, _CACHE.get('nb', NB), _CACHE.get('parts', 15), _CACHE.get('nb2', S_TOK // T2), _CACHE.get('nexp', 16))
    in_maps = _prep(inputs)
    res = run_bass_kernel_spmd(nc, in_maps, core_ids=list(range(8)))
    return np.stack([np.asarray(r["out"], dtype=np.float32) for r in res.results], axis=0)
```

```python
import numpy as np
import concourse.bass as bass
import concourse.mybir as mybir
from concourse.bass_utils import run_bass_kernel_spmd
from contextlib import ExitStack

F32 = mybir.dt.float32
BF16 = mybir.dt.bfloat16
AF = mybir.ActivationFunctionType
ALU = mybir.AluOpType

ENGS = ('pe', 'act', 'dve', 'pool', 'sp')
_CACHE = {}
OPT = {'la': 2, 'prefA': True, 'rawact': True}
NDS = 40

S_TOK = 4096
D = 1024
TB = 256
NB = S_TOK // TB
T2 = 1024
EPS = 1e-6
NEG = -30000.0

P_FB, P_DTB, P_ALOG, P_DSK, P_GMIX, P_GFFN, P_GPLE, P_CB, P_CW, P_BR, P_GSSD, P_GFIN, P_W = \
    0, 8, 16, 24, 32, 40, 48, 56, 64, 96, 128, 640, 1664
C_ID, C_TRI, C_ONE, C_NEG, C_BM, C_W = 0, 128, 256, 384, 512, 1536


class Op:
    __slots__ = ('eng', 'fn', 'deps', 'dma', 'ds', 'dval', 'sig', 'cnt', 'idx')


class _Rec:
    def __getattr__(self, name):
        def f(*a, **k):
            self.call = (name, a, k)
            return self
        return f


class Sched:
    def __init__(self, nc, es):
        self.nc = nc
        self.sem = {e: es.enter_context(nc.semaphore("s_" + e)) for e in ENGS}
        self.dsem = [es.enter_context(nc.semaphore("d%d" % i)) for i in range(NDS)]
        self.dval = [0] * NDS
        self.dlast = [None] * NDS
        self.dnext = {'pool': 0, 'sp': NDS // 2, 'act': NDS // 2}
        self.cnt = {e: 0 for e in ENGS}
        self.ops = []
        self.lastw = {}
        self.readers = {}
        self.nops = 0
        self.last_fp32 = None

    def add(self, eng, fn, r=(), w=(), dma=False):
        o = Op()
        isfp32 = False
        if fn is not None:
            rec = _Rec()
            fn(rec)
            c0 = rec.call
            if eng == 'pe' and c0[0] == 'matmul':
                lhsT = c0[1][1] if len(c0[1]) > 1 else c0[2].get('lhsT')
                isfp32 = (lhsT.dtype == F32)
            fn = (lambda e, c=rec.call: getattr(e, c[0])(*c[1], **c[2]))
        o.eng = eng; o.fn = fn; o.dma = dma; o.sig = False; o.cnt = None
        o.idx = len(self.ops)
        deps = []
        if eng == 'pe' and OPT.get('fp32fence', True) and fn is not None:
            lp = self.last_fp32
            if lp is not None and lp[1] != isfp32:
                deps.append((lp[0], True))
            self.last_fp32 = (o, isfp32)
        for k in r:
            lw = self.lastw.get(k)
            if lw is not None:
                deps.append((lw, True))
        for k in w:
            lw = self.lastw.get(k)
            if lw is not None:
                deps.append((lw, False))
            for rd in self.readers.get(k, ()):
                deps.append((rd, False))
        if dma:
            j = self.dnext[eng]
            half = NDS // 2
            base = 0 if eng == 'pool' else half
            nxt = base + (j - base + 1) % half
            for e2 in self.dnext:
                if (e2 == 'pool') == (eng == 'pool'):
                    self.dnext[e2] = nxt
            o.ds = j
            self.dval[j] += 16
            o.dval = self.dval[j]
            if self.dlast[j] is not None:
                deps.append((self.dlast[j], True))
            self.dlast[j] = o
        else:
            o.ds = None; o.dval = None
        dd = {}
        for d, raw in deps:
            if d is o:
                continue
            if d.dma or d.eng != eng or raw or eng != 'pe':
                dd[id(d)] = d
        o.deps = list(dd.values())
        for k in r:
            self.readers.setdefault(k, []).append(o)
        for k in w:
            self.lastw[k] = o
            self.readers[k] = []
        self.ops.append(o)
        return o

    def pe(self, fn, r=(), w=()): return self.add('pe', fn, r, w)
    def act(self, fn, r=(), w=()): return self.add('act', fn, r, w)
    def dve(self, fn, r=(), w=()): return self.add('dve', fn, r, w)
    def pool(self, fn, r=(), w=()): return self.add('pool', fn, r, w)

    def dma(self, eng, out, in_, r=(), w=()):
        return self.add(eng, lambda e: e.dma_start(out=out, in_=in_), r, w, dma=True)

    def emit(self):
        nc = self.nc
        ops = self.ops
        for o in ops:
            for d in o.deps:
                if not d.dma:
                    d.sig = True
        last = {}
        for o in ops:
            if not o.dma and o.fn is not None:
                last[o.eng] = o
        for o in last.values():
            o.sig = True
        cnt = dict(self.cnt)
        for o in ops:
            if not o.dma and o.sig:
                cnt[o.eng] += 1
                o.cnt = cnt[o.eng]
        endcnt = cnt
        per = {e: [o for o in ops if o.eng == e] for e in ENGS}
        sem = self.sem; dsem = self.dsem
        dval_end = list(self.dval)

        def body(ename):
            def run(eng):
                known = {}
                for o in per[ename]:
                    for d in o.deps:
                        if d.dma:
                            key = ('d', d.ds); val = d.dval; s = dsem[d.ds]
                        else:
                            key = d.eng; val = d.cnt; s = sem[d.eng]
                        if known.get(key, 0) >= val:
                            continue
                        known[key] = val
                        eng.wait_ge(s, val)
                    if o.fn is None:
                        continue
                    ins = o.fn(eng)
                    if o.dma:
                        ins.then_inc(dsem[o.ds], 16)
                    elif o.sig:
                        ins.then_inc(sem[ename], 1)
                for f in ENGS:
                    if f != ename and endcnt[f] > 0:
                        eng.wait_ge(sem[f], endcnt[f])
                for j in range(NDS):
                    if dval_end[j] > 0:
                        eng.wait_ge(dsem[j], dval_end[j])
            return run

        with nc.Block() as block:
            block.tensor(body('pe'))
            block.scalar(body('act'))
            block.vector(body('dve'))
            block.gpsimd(body('pool'))
            block.sync(body('sp'))
        self.cnt = endcnt
        self.nops += len(ops)
        self.ops = []
        self.lastw = {}
        self.readers = {}
        self.dlast = [None] * NDS
        self.last_fp32 = None


def build(stage=3, nb=NB, parts=15, nb2=S_TOK // T2, nexp=16, p1=True):
    nc = bass.Bass("TRN2", target_bir_lowering=False)

    def din(name, shape):
        return nc.dram_tensor(name, shape, F32, kind="ExternalInput").ap()

    x = din("x", [S_TOK, D])
    p_in = din("p", [S_TOK, 256])
    w_in = din("w_in", [D, 3088])
    w_sm = din("w_sm", [D, 16])
    w_out = din("w_out", [D, D])
    w_rt = din("w_rt", [D, 20])
    w_g = din("w_g", [16, D, 512])
    w_u = din("w_u", [16, D, 512])
    w_d = din("w_d", [16, 512, D])
    w_pg = din("w_pg", [D, D])
    w_pp = din("w_pp", [256, D])
    pbd = din("pb", [128, P_W])
    cstd = din("cst", [128, C_W])
    out = nc.dram_tensor("out", [S_TOK, D], F32, kind="ExternalOutput").ap()
    if p1:
        h1d = nc.dram_tensor("h1d", [S_TOK, D], F32, kind="Internal").ap()
    else:
        h1d = din("h1dbg", [S_TOK, D])

    with ExitStack() as es0:
        S = Sched(nc, es0)

        with ExitStack() as es:
            if not p1:
                nb = 0
            def sb(name, shape, dt=F32):
                return es.enter_context(nc.sbuf_tensor("t_" + name, shape, dt))

            def psb(name):
                return es.enter_context(nc.psum_tensor(name, [128, 512], F32))

            B = [psb("B%d" % i) for i in range(8)]
            B0b = B[0][:].bitcast(BF16)
            B1b = B[1][:].bitcast(BF16)

            cst = sb("cst", [128, C_W - 128])
            pb = sb("pb", [128, P_GFIN])
            ident = sb("ident", [128, 128], BF16)
            negm = sb("negm", [128, 512], BF16)
            wsm = sb("wsm", [128, 8, 16], BF16)
            a_b = sb("a_b", [128, 8])
            KT = [sb("KT%d" % h, [96, S_TOK], BF16) for h in range(8)]
            Vx = sb("Vx", [128, 32, 8, 65], BF16)
            Fk = sb("Fk", [128, 32, 8])
            carF = sb("carF", [128, 8])
            carFT = sb("carFT", [8, 1])
            S32 = sb("S32", [128, 512])
            Sbf = sb("Sbf", [128, 512], BF16)
            halo = sb("halo", [128, 8, 3])
            wbuf = [sb("wbuf%d" % i, [128, 4096], BF16) for i in range(3)]
            xt = [sb("xt%d" % i, [128, D]) for i in range(2)]
            xn2 = [sb("xn2%d" % i, [128, D], BF16) for i in range(2)]
            xr = [sb("xr%d" % i, [128, D]) for i in range(2)]
            ssqA = [sb("ssqA%d" % i, [128, 1]) for i in range(2)]; rstdA = [sb("rstdA%d" % i, [128, 1]) for i in range(2)]
            Osb8all = sb("Osb8all", [65, 4 * TB])
            ssq2 = sb("ssq2", [128, 1]); rstd2 = sb("rstd2", [128, 1])
            hnT = sb("hnT", [128, 8, TB], BF16)
            raw = [sb("raw%d" % i, [128, TB + 3]) for i in range(2)]
            cacc = [sb("cacc%d" % i, [128, TB]) for i in range(2)]
            xbcT = sb("xbcT", [128, 8, TB], BF16)
            zs = [sb("zs%d" % i, [128, 512], BF16) for i in range(2)]
            QT = [sb("QT%d" % h, [96, TB], BF16) for h in range(8)]
            tmpc = []
            for tt_ in range(2):
                tmpc.append({
                    'sm': sb("sm%d" % tt_, [128, 16]), 'sp': sb("sp%d" % tt_, [128, 16]), 'dt_t': sb("dt_t%d" % tt_, [128, 8]),
                    'ea': sb("ea%d" % tt_, [128, 8]), 'cd_b': sb("cd_b%d" % tt_, [128, 8]), 'tot_sb': sb("tot_sb%d" % tt_, [128, 8]),
                    'dd': sb("dd%d" % tt_, [128, 8]), 'dst': sb("dst%d" % tt_, [128, 8]),
                    'nacsT': sb("nacsT%d" % tt_, [96, 128]), 'BDacs': sb("BDacs%d" % tt_, [96, 1024])})
            rTb = sb("rTb", [8, TB], BF16)
            xsB = sb("xsB", [128, 768], BF16)
            xdt = sb("xdt", [128, 512], BF16); xsD = sb("xsD", [128, 512], BF16); xdtd = sb("xdtd", [128, 512], BF16)
            dec1 = sb("dec1", [128, 512]); dec = [dec1, dec1]; MT = [sb("MT%d" % g, [128, 512], BF16) for g in range(2)]
            y1 = sb("y1", [128, 512]); gy = sb("gy", [128, 512]); yn = sb("yn", [128, 512], BF16)
            mixT = sb("mixT", [128, 4, TB], BF16)
            PT = [sb("PT%d" % i, [128, TB], BF16) for i in range(3)]
            yTall = sb("yTall", [96, 8, TB], BF16)

            S.dma('sp', cst[:], cstd[:, 128:C_W], w=['cst'])
            S.dma('sp', pb[:], pbd[:, 0:P_GFIN], w=['pb'])
            S.dma('pool', wsm[:], w_sm.rearrange("(kc p) c -> p kc c", p=128), w=['wsm'])
            S.dma('pool', ident[:], cstd[:, C_ID:C_ID + 128], w=['ident'])
            for i in range(4):
                S.dve(lambda e, i=i: e.tensor_copy(out=negm[:, i * 128:(i + 1) * 128], in_=cst[:, C_NEG - 128:C_NEG]),
                      r=['cst'], w=['negm'])
            tri = cst[:, C_TRI - 128:C_TRI]
            ones = cst[:, C_ONE - 128:C_ONE]
            bmask = cst[0:8, C_BM - 128:C_BM - 128 + 1024]
            bmask96 = cst[0:96, C_BM - 128:C_BM - 128 + 1024]
            S.act(lambda e: e.activation(out=a_b[:], in_=pb[:, P_ALOG:P_ALOG + 8], func=AF.Exp), r=['pb'], w=['a_b'])
            S.dve(lambda e: e.tensor_scalar(out=a_b[:], in0=a_b[:], scalar1=-1.0, scalar2=None, op0=ALU.mult),
                  r=['a_b'], w=['a_b'])
            for h in range(8):
                S.pool(lambda e, h=h: e.memset(KT[h][64:96, :], 0.0), w=['KT%d' % h])
                S.pool(lambda e, h=h: e.memset(KT[h][64:65, :], 1.0), w=['KT%d' % h])
                S.pool(lambda e, h=h: e.memset(QT[h][64:96, :], 0.0), w=['QT%d' % h])
            S.pool(lambda e: e.memset(Vx[:], 1.0), w=['Vx'])
            S.pool(lambda e: e.memset(carF[:], 0.0), w=['carF'])
            S.pool(lambda e: e.memset(carFT[:], 0.0), w=['carFT'])
            S.pool(lambda e: e.memset(S32[:], 0.0), w=['S32'])
            S.pool(lambda e: e.memset(Sbf[:], 0.0), w=['Sbf'])
            S.pool(lambda e: e.memset(halo[:], 0.0), w=['halo'])
            for tt_ in range(2):
                S.pool(lambda e: e.memset(tmpc[tt_]['nacsT'][:], 0.0), w=['nacsT_%d' % tt_])
                S.pool(lambda e: e.memset(tmpc[tt_]['BDacs'][:], 0.0), w=['BDacs_%d' % tt_])
            S.pool(lambda e: e.memset(yTall[64:96, :, :], 0.0), w=['yT'])

            wstate = {'n': 0}

            def wsrc(ci):
                c = ci % 9
                if c < 6:
                    c0 = [0, 512, 1024, 1544, 2056, 2568][c]
                    return (128, w_in[:, c0:c0 + 512].rearrange("(kc p) c -> p kc c", p=128), [128, 8, 512])
                if c == 6:
                    return (128, w_out[0:512, :].rearrange("(kc p) c -> p kc c", p=128), [128, 4, 1024])
                hh = (c - 7) * 4
                return (64, w_out[512 + hh * 64:512 + (hh + 4) * 64, :].rearrange("(h p) c -> p h c", p=64), [64, 4, 1024])

            def wload():
                ci = wstate['n']
                if ci >= NB * 9:
                    return
                wstate['n'] += 1
                bi = ci % 3
                npart, src, shp = wsrc(ci)
                dstv = wbuf[bi][0:npart, :].rearrange("p (a b) -> p a b", a=shp[1])
                S.dma('pool', dstv, src, w=['wbuf%d' % bi])

            def wview(ci):
                bi = ci % 3
                npart, src, shp = wsrc(ci)
                return wbuf[bi][0:npart, :].rearrange("p (a b) -> p a b", a=shp[1]), 'wbuf%d' % bi

            wload(); wload(); wload()

            def secA_pre(bb):
                for tt in range(2):
                    r0 = bb * TB + tt * 128
                    X = xt[tt]; kx = 'xt%d' % tt
                    S.dma('sp', X[:], x[r0:r0 + 128, :], w=[kx])
                    S.act(lambda e: e.activation(out=xn2[tt][:], in_=X[:], func=AF.Square, accum_out=ssqA[tt][:]),
                          r=[kx], w=['xn2%d' % tt, 'ssqA%d' % tt])
                    S.act(lambda e: e.activation(out=rstdA[tt][:], in_=ssqA[tt][:], func=AF.Sqrt, scale=1.0 / D, bias=EPS),
                          r=['ssqA%d' % tt], w=['rstdA%d' % tt])
                    S.dve(lambda e: e.reciprocal(out=rstdA[tt][:], in_=rstdA[tt][:]), r=['rstdA%d' % tt], w=['rstdA%d' % tt])
                    S.pool(lambda e: e.tensor_scalar(out=xn2[tt][:], in0=X[:], scalar1=rstdA[tt][:, 0:1], scalar2=1.0,
                                                     op0=ALU.mult, op1=ALU.mult), r=[kx, 'rstdA%d' % tt], w=['xn2%d' % tt])

            def secA_pe(bb):
                for tt in range(2):
                    Bt = (B1b, B0b)[tt]; kb = ('B1', 'B0')[tt]
                    for kc in range(8):
                        S.pe(lambda e, kc=kc: e.transpose(Bt[:, kc * 128:(kc + 1) * 128], xn2[tt][:, kc * 128:(kc + 1) * 128], ident[:]),
                             r=['xn2%d' % tt, 'ident'], w=[kb])
                    S.dve(lambda e: e.tensor_tensor(
                        out=hnT[:, :, tt * 128:(tt + 1) * 128],
                        in0=Bt.rearrange("p (a b) -> p a b", a=8),
                        in1=pb[:, P_GMIX:P_GMIX + 8].unsqueeze(2).to_broadcast([128, 8, 128]), op=ALU.mult),
                        r=[kb, 'pb'], w=['hnT'])

            for b in range(nb):
                t0 = b * TB
                if b == 0 or not OPT['prefA']:
                    secA_pre(b); secA_pe(b)
                for tt in range(2):
                    r0 = t0 + tt * 128
                    S.dma('sp', xr[tt][:], x[r0:r0 + 128, :], w=['xr%d' % tt])

                ci0 = b * 9
                for tt in range(2):
                    for kc in range(8):
                        S.pe(lambda e, tt=tt, kc=kc: e.matmul(B[4][:, 288 + tt * 16:304 + tt * 16], hnT[:, kc, tt * 128:(tt + 1) * 128],
                                                              wsm[:, kc, :], start=(kc == 0), stop=(kc == 7)),
                             r=['hnT', 'wsm'], w=['B4'])
                wz, kz = wview(ci0 + 0)
                for tt in range(2):
                    pz = B[2 + tt]; kp = 'B%d' % (2 + tt)
                    for kc in range(8):
                        S.pe(lambda e, tt=tt, kc=kc, pz=pz: e.matmul(pz[:, :], hnT[:, kc, tt * 128:(tt + 1) * 128], wz[:, kc, :],
                                                                   start=(kc == 0), stop=(kc == 7)),
                             r=['hnT', kz], w=[kp])
                    S.act(lambda e, tt=tt, pz=pz: e.activation(out=zs[tt][:], in_=pz[:, :], func=AF.Silu),
                          r=[kp], w=['zs%d' % tt])
                for tt in range(2):
                    kt = 2 * b + tt
                    tk = slice(tt * 128, (tt + 1) * 128)
                    psm = B[4][:, 288 + tt * 16:304 + tt * 16]
                    T = tmpc[tt]
                    sm, sp_, dt_t, ea, cd_b, tot_sb, dd, dst, nacsT, BDacs = (T[k] for k in
                        ('sm', 'sp', 'dt_t', 'ea', 'cd_b', 'tot_sb', 'dd', 'dst', 'nacsT', 'BDacs'))
                    K_ = lambda n, tt=tt: '%s_%d' % (n, tt)
                    S.dve(lambda e: e.scalar_tensor_tensor(out=sm[:, 0:8], in0=psm[:, 0:8], scalar=-1.0,
                                                           in1=pb[:, P_FB:P_FB + 8], op0=ALU.mult, op1=ALU.subtract),
                          r=['B4', 'pb'], w=[K_('sm')])
                    S.dve(lambda e: e.tensor_tensor(out=sm[:, 8:16], in0=psm[:, 8:16], in1=pb[:, P_DTB:P_DTB + 8], op=ALU.add),
                          r=['B4', 'pb'], w=[K_('sm')])
                    S.act(lambda e: e.activation(out=sp_[:], in_=sm[:], func=AF.Exp), r=[K_('sm')], w=[K_('sp')])
                    S.act(lambda e: e.activation(out=sp_[:], in_=sp_[:], func=AF.Ln, bias=1.0), r=[K_('sp')], w=[K_('sp')])
                    S.dve(lambda e: e.tensor_copy(out=dt_t[:], in_=sp_[:, 8:16]), r=[K_('sp')], w=[K_('dt_t')])
                    S.dve(lambda e: e.tensor_tensor(out=sp_[:, 8:16], in0=sp_[:, 8:16], in1=a_b[:], op=ALU.mult),
                          r=[K_('sp'), 'a_b'], w=[K_('sp')])
                for tt in range(2):
                    kt = 2 * b + tt
                    tk = slice(tt * 128, (tt + 1) * 128)
                    psm = B[4][:, 288 + tt * 16:304 + tt * 16]
                    T = tmpc[tt]
                    sm, sp_, dt_t, ea, cd_b, tot_sb, dd, dst, nacsT, BDacs = (T[k] for k in
                        ('sm', 'sp', 'dt_t', 'ea', 'cd_b', 'tot_sb', 'dd', 'dst', 'nacsT', 'BDacs'))
                    K_ = lambda n, tt=tt: '%s_%d' % (n, tt)
                    S.pe(lambda e: e.matmul(B[4][:, 0:16], tri, sp_[:], start=True, stop=True), r=[K_('sp'), 'cst'], w=['B4'])
                    S.pe(lambda e: e.matmul(B[4][:, 16:32], ones, sp_[:], start=True, stop=True), r=[K_('sp'), 'cst'], w=['B4'])
                    S.pe(lambda e: e.matmul(B[4][0:8, 32:160], sp_[:, 8:16], tri, start=True, stop=True), r=[K_('sp'), 'cst'], w=['B4'])
                    S.pe(lambda e: e.matmul(B[4][0:8, 160:288], sp_[:, 0:8], tri, start=True, stop=True), r=[K_('sp'), 'cst'], w=['B4'])
                    S.dve(lambda e: e.tensor_tensor(out=Fk[:, kt, :], in0=B[4][:, 0:8], in1=carF[:], op=ALU.add),
                          r=['B4', 'carF'], w=['Fk'])
                    S.dve(lambda e: e.tensor_scalar(out=rTb[:, tk], in0=B[4][0:8, 160:288], scalar1=carFT[0:8, 0:1],
                                                    scalar2=-1.0, op0=ALU.add, op1=ALU.mult),
                          r=['B4', 'carFT'], w=['rTb'])
                    S.dve(lambda e: e.tensor_tensor(out=carF[:], in0=B[4][:, 16:24], in1=carF[:], op=ALU.add),
                          r=['B4', 'carF'], w=['carF'])
                    S.dve(lambda e: e.tensor_tensor(out=carFT[:], in0=B[4][0:8, 287:288], in1=carFT[:], op=ALU.add),
                          r=['B4', 'carFT'], w=['carFT'])
                    S.act(lambda e: e.activation(out=ea[:], in_=B[4][:, 8:16], func=AF.Exp), r=['B4'], w=[K_('ea')])
                    S.act(lambda e: e.activation(out=cd_b[:], in_=B[4][:, 24:32], func=AF.Exp), r=['B4'], w=[K_('cd_b')])
                    S.dve(lambda e: e.tensor_copy(out=tot_sb[:], in_=B[4][:, 24:32]), r=['B4'], w=[K_('tot_sb')])
                    S.dve(lambda e: e.scalar_tensor_tensor(out=dd[:], in0=B[4][:, 8:16], scalar=-1.0, in1=tot_sb[:],
                                                           op0=ALU.mult, op1=ALU.add), r=['B4', K_('tot_sb')], w=[K_('dd')])
                    S.act(lambda e: e.activation(out=dst[:], in_=dd[:], func=AF.Exp), r=[K_('dd')], w=[K_('dst')])
                    S.dve(lambda e: e.tensor_scalar(out=nacsT[0:8, :], in0=B[4][0:8, 32:160], scalar1=-1.0, scalar2=None, op0=ALU.mult),
                          r=['B4'], w=[K_('nacsT')])
                    S.dve(lambda e: e.tensor_tensor(out=BDacs[0:8, :].rearrange("k (r l) -> k r l", r=8),
                                                    in0=B[4][0:8, 32:160].unsqueeze(1).to_broadcast([8, 8, 128]),
                                                    in1=bmask.rearrange("k (r l) -> k r l", r=8), op=ALU.mult),
                          r=['B4', 'cst'], w=[K_('BDacs')])
                for h in range(8):
                    S.dma('sp', QT[h][64:65, :], rTb[h:h + 1, :], r=['rTb'], w=['QT%d' % h])

                wload()
                pend_silu = []
                for fc in range(8):
                    wx, kw = wview(ci0 + 1 + fc // 4)
                    pbk = B[(2, 3, 6, 7)[fc % 4]]; kp = 'B%d' % (2, 3, 6, 7)[fc % 4]
                    R = raw[fc % 2]; kr = 'raw%d' % (fc % 2)
                    A = cacc[fc % 2]; ka = 'cacc%d' % (fc % 2)
                    for kc in range(8):
                        S.pe(lambda e, fc=fc, kc=kc, pbk=pbk, wx=wx: e.matmul(
                            pbk[:, 0:TB], wx[:, kc, (fc % 4) * 128:(fc % 4 + 1) * 128], hnT[:, kc, :],
                            start=(kc == 0), stop=(kc == 7)), r=['hnT', kw], w=[kp])
                    if fc == 3:
                        wload()
                    S.pool(lambda e, fc=fc, R=R: e.tensor_copy(out=R[:, 0:3], in_=halo[:, fc, :]), r=['halo'], w=[kr + 'h'])
                    if OPT['rawact']:
                        S.act(lambda e, pbk=pbk, R=R: e.activation(out=R[:, 3:3 + TB], in_=pbk[:, 0:TB], func=AF.Copy),
                              r=[kp], w=[kr])
                    else:
                        S.dve(lambda e, pbk=pbk, R=R: e.tensor_copy(out=R[:, 3:3 + TB], in_=pbk[:, 0:TB]),
                              r=[kp], w=[kr])
                    S.pool(lambda e, fc=fc, R=R: e.tensor_copy(out=halo[:, fc, :], in_=R[:, TB:TB + 3]), r=[kr], w=['halo'])
                    for (fc_, A_, ka_) in pend_silu:
                        S.act(lambda e: e.activation(out=xbcT[:, fc_, :], in_=A_[:], func=AF.Silu), r=[ka_], w=['xbcT'])
                    pend_silu = []
                    cw0 = P_CW + fc * 4
                    S.dve(lambda e, R=R, A=A, cw0=cw0, fc=fc: e.tensor_scalar(
                        out=A[:], in0=R[:, 0:TB], scalar1=pb[:, cw0:cw0 + 1], scalar2=pb[:, P_CB + fc:P_CB + fc + 1],
                        op0=ALU.mult, op1=ALU.add), r=[kr, kr + 'h', 'pb'], w=[ka])
                    for k in range(1, 4):
                        S.dve(lambda e, R=R, A=A, cw0=cw0, k=k: e.scalar_tensor_tensor(
                            out=A[:], in0=R[:, k:k + TB], scalar=pb[:, cw0 + k:cw0 + k + 1], in1=A[:],
                            op0=ALU.mult, op1=ALU.add), r=[kr, kr + 'h', ka, 'pb'], w=[ka])
                    pend_silu.append((fc, A, ka))
                for (fc_, A_, ka_) in pend_silu:
                    S.act(lambda e: e.activation(out=xbcT[:, fc_, :], in_=A_[:], func=AF.Silu), r=[ka_], w=['xbcT'])
                wload()
                wq, kwq = wview(ci0 + 3)
                wk, kwk = wview(ci0 + 4)
                for h in range(8):
                    pbk = B[(2, 3, 6, 7)[h % 4]]; kp = 'B%d' % (2, 3, 6, 7)[h % 4]
                    for kc in range(8):
                        S.pe(lambda e, h=h, kc=kc, pbk=pbk: e.matmul(pbk[0:64, 0:TB], wq[:, kc, h * 64:(h + 1) * 64], hnT[:, kc, :],
                                                                   start=(kc == 0), stop=(kc == 7)),
                             r=['hnT', kwq], w=[kp])
                    S.dve(lambda e, h=h, pbk=pbk: e.tensor_scalar(out=QT[h][0:64, :], in0=pbk[0:64, 0:TB], scalar1=0.125,
                                                                  scalar2=None, op0=ALU.mult),
                          r=[kp], w=['QT%d' % h])
                wload()
                for h in range(8):
                    pbk = B[(2, 3, 6, 7)[h % 4]]; kp = 'B%d' % (2, 3, 6, 7)[h % 4]
                    for kc in range(8):
                        S.pe(lambda e, h=h, kc=kc, pbk=pbk: e.matmul(pbk[0:64, 0:TB], wk[:, kc, h * 64:(h + 1) * 64], hnT[:, kc, :],
                                                                   start=(kc == 0), stop=(kc == 7)),
                             r=['hnT', kwk], w=[kp])
                    S.dve(lambda e, h=h, pbk=pbk: e.tensor_copy(out=KT[h][0:64, t0:t0 + TB], in_=pbk[0:64, 0:TB]),
                          r=[kp], w=['KT%d' % h])
                wload()
                wv, kwv = wview(ci0 + 5)
                for tt in range(2):
                    pv = B[2 + tt]; kp = 'B%d' % (2 + tt)
                    kt = 2 * b + tt
                    for kc in range(8):
                        S.pe(lambda e, tt=tt, kc=kc, pv=pv: e.matmul(pv[:, :], hnT[:, kc, tt * 128:(tt + 1) * 128], wv[:, kc, :],
                                                                   start=(kc == 0), stop=(kc == 7)),
                             r=['hnT', kwv], w=[kp])
                    S.dve(lambda e, kt=kt, pv=pv: e.tensor_copy(out=Vx[:, kt, :, 0:64],
                                                                in_=pv[:, :].rearrange("p (h d) -> p h d", h=8)),
                          r=[kp], w=['Vx'])
                wload()

                def ssd_gen():
                    for tt in range(2):
                        tk = slice(tt * 128, (tt + 1) * 128)
                        T = tmpc[tt]
                        dt_t, ea, cd_b, dst, nacsT, BDacs = (T[k] for k in ('dt_t', 'ea', 'cd_b', 'dst', 'nacsT', 'BDacs'))
                        K_ = lambda n, tt=tt: '%s_%d' % (n, tt)
                        for fc in range(6):
                            S.pe(lambda e, fc=fc: e.transpose(B1b[:, fc * 128:(fc + 1) * 128], xbcT[:, fc, tk], ident[:]),
                                 r=['xbcT', 'ident'], w=['B1'])
                        S.dve(lambda e: e.tensor_copy(out=xsB[:], in_=B1b[:, 0:768]), r=['B1'], w=['xsB'])
                        xs3 = xsB[:, 0:512].rearrange("p (r d) -> p r d", r=8)
                        S.pool(lambda e: e.tensor_tensor(out=xdt[:].rearrange("p (r d) -> p r d", r=8), in0=xs3,
                                                         in1=dt_t[:].unsqueeze(2).to_broadcast([128, 8, 64]), op=ALU.mult),
                               r=['xsB', K_('dt_t')], w=['xdt'])
                        S.pool(lambda e: e.tensor_tensor(out=xsD[:].rearrange("p (r d) -> p r d", r=8), in0=xs3,
                                                         in1=pb[:, P_DSK:P_DSK + 8].unsqueeze(2).to_broadcast([128, 8, 64]), op=ALU.mult),
                               r=['xsB', 'pb'], w=['xsD'])
                        S.pool(lambda e: e.tensor_tensor(out=xdtd[:].rearrange("p (r d) -> p r d", r=8),
                                                         in0=xdt[:].rearrange("p (r d) -> p r d", r=8),
                                                         in1=dst[:].unsqueeze(2).to_broadcast([128, 8, 64]), op=ALU.mult),
                               r=['xdt', K_('dst')], w=['xdtd'])
                        yield
                        for g in range(2):
                            pcb = B[4][:, 352:480]
                            S.pe(lambda e: e.matmul(pcb, xbcT[:, 4 + g, tk], xbcT[:, 6 + g, tk], start=True, stop=True),
                                 r=['xbcT'], w=['B4'])
                            S.pe(lambda e: e.matmul(B[5][:, :], ones[0:96, :], BDacs[0:96, g * 512:(g + 1) * 512], start=True, stop=False),
                                 r=['cst', K_('BDacs')], w=['B5'])
                            S.pe(lambda e: e.matmul(B[5][:, :], nacsT[0:96, :], bmask96[:, g * 512:(g + 1) * 512], start=False, stop=False),
                                 r=['cst', K_('nacsT')], w=['B5'])
                            S.pe(lambda e: e.matmul(B[5][:, :], ident[:], negm[:], start=False, stop=True),
                                 r=['ident', 'negm'], w=['B5'])
                            S.act(lambda e: e.activation(out=dec[g][:], in_=B[5][:, :], func=AF.Exp), r=['B5'], w=['dec'])
                            S.dve(lambda e: e.tensor_tensor(
                                out=MT[g][:].rearrange("p (r l) -> p r l", r=4),
                                in0=pcb.unsqueeze(1).to_broadcast([128, 4, 128]),
                                in1=dec[g][:].rearrange("p (r l) -> p r l", r=4), op=ALU.mult),
                                r=['B4', 'dec'], w=['MT%d' % g])
                            yield
                        for g in range(2):
                            S.pe(lambda e, g=g: e.matmul(B[5][:, g * 256:(g + 1) * 256], xbcT[:, 6 + g, tk], Sbf[:, g * 256:(g + 1) * 256],
                                                         start=True, stop=True), r=['xbcT', 'Sbf'], w=['B5'])
                        S.dve(lambda e: e.tensor_tensor(out=y1[:].rearrange("p (r d) -> p r d", r=8),
                                                        in0=B[5][:, :].rearrange("p (r d) -> p r d", r=8),
                                                        in1=ea[:].unsqueeze(2).to_broadcast([128, 8, 64]), op=ALU.mult),
                              r=['B5', K_('ea')], w=['y1'])
                        yield
                        for r_ in range(8):
                            g = r_ // 4
                            S.pe(lambda e, r_=r_, g=g: e.matmul(B[5][:, r_ * 64:(r_ + 1) * 64], MT[g][:, (r_ % 4) * 128:(r_ % 4 + 1) * 128],
                                                                xdt[:, r_ * 64:(r_ + 1) * 64], start=(r_ == 0), stop=False, skip_group_check=True),
                                 r=['MT%d' % g, 'xdt'], w=['B5'])
                        S.pe(lambda e: e.matmul(B[5][:, :], ident[:], xsD[:], start=False, stop=True, skip_group_check=True),
                             r=['ident', 'xsD'], w=['B5'])
                        S.dve(lambda e: e.tensor_tensor(out=y1[:], in0=B[5][:, :], in1=y1[:], op=ALU.add), r=['B5', 'y1'], w=['y1'])
                        S.dve(lambda e: e.tensor_tensor(out=gy[:], in0=y1[:], in1=zs[tt][:], op=ALU.mult),
                              r=['y1', 'zs%d' % tt], w=['gy'])
                        S.act(lambda e: e.activation(out=yn[:], in_=gy[:], func=AF.Square, accum_out=ssq2[:]),
                              r=['gy'], w=['yn', 'ssq2'])
                        S.act(lambda e: e.activation(out=rstd2[:], in_=ssq2[:], func=AF.Sqrt, scale=1.0 / 512, bias=EPS),
                              r=['ssq2'], w=['rstd2'])
                        S.dve(lambda e: e.reciprocal(out=rstd2[:], in_=rstd2[:]), r=['rstd2'], w=['rstd2'])
                        S.dve(lambda e: e.scalar_tensor_tensor(out=yn[:], in0=gy[:], scalar=rstd2[:, 0:1], in1=pb[:, P_GSSD:P_GSSD + 512],
                                                               op0=ALU.mult, op1=ALU.mult), r=['gy', 'rstd2', 'pb'], w=['yn'])
                        yield
                        for g in range(2):
                            S.pe(lambda e, g=g: e.matmul(B[5][:, g * 256:(g + 1) * 256], xsB[:, 512 + g * 128:512 + (g + 1) * 128],
                                                         xdtd[:, g * 256:(g + 1) * 256], start=True, stop=True),
                                 r=['xsB', 'xdtd'], w=['B5'])
                        S.dve(lambda e: e.tensor_tensor(out=S32[:].rearrange("p (r d) -> p r d", r=8),
                                                        in0=S32[:].rearrange("p (r d) -> p r d", r=8),
                                                        in1=cd_b[:].unsqueeze(2).to_broadcast([128, 8, 64]), op=ALU.mult),
                              r=['S32', K_('cd_b')], w=['S32'])
                        S.dve(lambda e: e.tensor_tensor(out=S32[:], in0=B[5][:, :], in1=S32[:], op=ALU.add),
                              r=['B5', 'S32'], w=['S32'])
                        S.pool(lambda e: e.tensor_copy(out=Sbf[:], in_=S32[:]), r=['S32'], w=['Sbf'])
                        yield
                        for c in range(4):
                            S.pe(lambda e, c=c: e.transpose(B1b[:, c * 128:(c + 1) * 128], yn[:, c * 128:(c + 1) * 128], ident[:]),
                                 r=['yn', 'ident'], w=['B1'])
                        S.dve(lambda e: e.tensor_copy(out=mixT[:, :, tk], in_=B1b[:, 0:512].rearrange("p (c l) -> p c l", c=4)),
                              r=['B1'], w=['mixT'])
                        yield

                def attn_gen():
                    nkt = 2 * b + 2
                    tiles = [(h, kt) for h in range(8) for kt in range(nkt)]

                    def qk(i):
                        h, kt = tiles[i]
                        dg = kt - 2 * b
                        c0 = 0 if dg < 0 else dg * 128
                        bsel = (2, 7, 0)[i % 3] if OPT['la'] == 2 else (2, 7)[i % 2]
                        pS = B[bsel][:, 0:TB]; kps = 'B%d' % bsel
                        ks_ = slice(kt * 128, (kt + 1) * 128)
                        if dg < 0:
                            S.pe(lambda e: e.matmul(pS[:, 0:TB], KT[h][0:96, ks_], QT[h][0:96, 0:TB], start=True, stop=True),
                                 r=['KT%d' % h, 'QT%d' % h], w=[kps])
                        else:
                            S.pe(lambda e: e.matmul(pS[:, c0:c0 + 128], ident[:], negm[:, 0:128], start=True, stop=False),
                                 r=['ident', 'negm'], w=[kps])
                            S.pe(lambda e: e.matmul(pS[:, c0:c0 + 128], KT[h][0:96, ks_], QT[h][0:96, c0:c0 + 128], start=False, stop=True),
                                 r=['KT%d' % h, 'QT%d' % h], w=[kps])
                            if c0 + 128 < TB:
                                S.pe(lambda e: e.matmul(pS[:, c0 + 128:TB], KT[h][0:96, ks_], QT[h][0:96, c0 + 128:TB], start=True, stop=True),
                                     r=['KT%d' % h, 'QT%d' % h], w=[kps])
                        P_ = PT[i % 3]; kpt = 'PT%d' % (i % 3)
                        S.act(lambda e: e.activation(out=P_[:, c0:TB], in_=pS[:, c0:TB], func=AF.Exp, bias=Fk[:, kt, h:h + 1]),
                              r=[kps, 'Fk'], w=[kpt])

                    def pv(i):
                        h, kt = tiles[i]
                        dg = kt - 2 * b
                        c0 = 0 if dg < 0 else dg * 128
                        P_ = PT[i % 3]; kpt = 'PT%d' % (i % 3)
                        ob = (3, 6)[h % 2]
                        pO = B[ob][0:65, 0:TB]
                        S.pe(lambda e: e.matmul(pO[:, c0:TB], Vx[:, kt, h, :], P_[:, c0:TB], start=(kt == 0), stop=(kt == nkt - 1),
                                                skip_group_check=True), r=['Vx', kpt], w=['B%d' % ob])
                        if kt == nkt - 1:
                            S.act(lambda e: e.activation(out=Osb8all[:, (h % 4) * TB:(h % 4 + 1) * TB], in_=pO, func=AF.Copy),
                                  r=['B%d' % ob], w=['Osb8'])
                            if h % 4 == 3:
                                S.dve(lambda e: e.reciprocal(out=Osb8all[64:65, :], in_=Osb8all[64:65, :]), r=['Osb8'], w=['Osb8'])
                                pend.append([h - 3, i + min(12, nkt - 1)])

                    pend = []

                    def normb(h0):
                        for pr in range(2):
                            cs2 = slice(pr * 2 * TB, (pr + 1) * 2 * TB)
                            S.pe(lambda e: e.matmul(B[0][0:64, :], cst[64:65, C_ONE - 128:C_ONE - 64], Osb8all[64:65, cs2], start=True, stop=True),
                                 r=['cst', 'Osb8'], w=['B0'])
                            S.dve(lambda e: e.tensor_tensor(out=yTall[0:64, h0 + 2 * pr:h0 + 2 * pr + 2, :],
                                                            in0=Osb8all[0:64, cs2].rearrange("p (a b) -> p a b", a=2),
                                                            in1=B[0][0:64, :].rearrange("p (a b) -> p a b", a=2), op=ALU.mult),
                                  r=['Osb8', 'B0'], w=['yT'])

                    if parts & 4:
                        LA = OPT['la']
                        for i0_ in range(min(LA, len(tiles))):
                            qk(i0_)
                        for i in range(len(tiles)):
                            if i + LA < len(tiles):
                                qk(i + LA)
                            pv(i)
                            if pend and i >= pend[0][1] and i + 1 < len(tiles):
                                normb(pend.pop(0)[0])
                            yield
                        yield 'tail'
                        normb(pend.pop(0)[0])
                        yield

                gs = ssd_gen() if parts & 2 else iter(())
                ga = attn_gen()
                n_attn = 8 * (2 * b + 2)
                n_ssd = 14
                every = max(1, (3 * n_attn) // (4 * (n_ssd + 1)))
                ia = 0
                pre_done = False
                ssd_done = False; attn_done = False
                pe_done = False
                while not (ssd_done and attn_done):
                    if not attn_done:
                        try:
                            tag = next(ga); ia += 1
                            if tag == 'tail' and b + 1 < nb and OPT['prefA']:
                                if not pre_done:
                                    secA_pre(b + 1); pre_done = True
                                secA_pe(b + 1); pe_done = True
                        except StopIteration:
                            attn_done = True
                    if OPT['prefA'] and not pre_done and b + 1 < nb and (attn_done or ia >= n_attn // 2):
                        secA_pre(b + 1)
                        pre_done = True
                    if not ssd_done and (attn_done or ia % every == 0):
                        try:
                            next(gs)
                        except StopIteration:
                            ssd_done = True
                if b + 1 < nb and OPT['prefA'] and not pe_done:
                    if not pre_done:
                        secA_pre(b + 1)
                    secA_pe(b + 1)

                wo_s, kws = wview(ci0 + 6)
                wo_f0, kwf0 = wview(ci0 + 7)
                wo_f1, kwf1 = wview(ci0 + 8)
                wo_f0 = wbuf[(ci0 + 7) % 3][0:96, :].rearrange("p (a b) -> p a b", a=4)
                wo_f1 = wbuf[(ci0 + 8) % 3][0:96, :].rearrange("p (a b) -> p a b", a=4)
                combos = [(tt, hf) for tt in range(2) for hf in range(2)]
                fbank = (2, 3, 6, 7)
                for ci_, (tt, hf) in enumerate(combos):
                    po = B[fbank[ci_]]; kp = 'B%d' % fbank[ci_]
                    tk = slice(tt * 128, (tt + 1) * 128); cs = slice(hf * 512, (hf + 1) * 512)
                    for c in range(4):
                        S.pe(lambda e, c=c: e.matmul(po[:, :], mixT[:, c, tk], wo_s[:, c, cs], start=(c == 0), stop=False),
                             r=['mixT', kws], w=[kp])
                wload()
                for (wf, kwf, hh0) in ((wo_f0, kwf0, 0), (wo_f1, kwf1, 4)):
                    for ci_, (tt, hf) in enumerate(combos):
                        po = B[fbank[ci_]]; kp = 'B%d' % fbank[ci_]
                        tk = slice(tt * 128, (tt + 1) * 128); cs = slice(hf * 512, (hf + 1) * 512)
                        for h in range(hh0, hh0 + 4):
                            S.pe(lambda e, h=h: e.matmul(po[:, :], yTall[0:96, h, tk], wf[0:96, h % 4, cs], start=False, stop=(h == 7)),
                                 r=['yT', kwf], w=[kp])
                    wload()
                for ci_, (tt, hf) in enumerate(combos):
                    po = B[fbank[ci_]]; kp = 'B%d' % fbank[ci_]
                    cs = slice(hf * 512, (hf + 1) * 512)
                    X = xr[tt]; kx = 'xr%d' % tt
                    S.dve(lambda e: e.tensor_tensor(out=X[:, cs], in0=po[:, :], in1=X[:, cs], op=ALU.add), r=[kp, kx], w=[kx])
                for tt in range(2):
                    r0 = t0 + tt * 128
                    S.dma('sp', h1d[r0:r0 + 128, :], xr[tt][:], r=['xr%d' % tt], w=['h1d'])

            S.emit()
        if stage == 1:
            with ExitStack() as es:
                for i in range(nb):
                    S.dma('sp', out[i * 256:(i + 1) * 256, :], h1d[i * 256:(i + 1) * 256, :], r=['h1d'], w=['out'])
                S.emit()
            return nc
        phase2(nc, S, h1d, p_in, w_rt, w_g, w_u, w_d, w_pg, w_pp, pbd, cstd, out, nb2, nexp)
    return nc


def phase2(nc, S, h1d, p_in, w_rt, w_g, w_u, w_d, w_pg, w_pp, pbd, cstd, out, nb2, nexp):
    NT = T2 // 128
    AXX = mybir.AxisListType.X
    with ExitStack() as es:
        def sb(name, shape, dt=F32):
            return es.enter_context(nc.sbuf_tensor("u_" + name, shape, dt))

        B = [es.enter_context(nc.psum_tensor("C%d" % i, [128, 512], F32)) for i in range(8)]
        B0b = B[0][:].bitcast(BF16)
        identf = sb("identf", [128, 128]); pb = sb("pb", [128, P_W])
        ident = sb("ident", [128, 128], BF16)
        wpg = sb("wpg", [128, 8, 1024], BF16); wpp = sb("wpp", [128, 2, 1024], BF16)
        wrt = sb("wrt", [128, 8, 20], BF16)
        eg = [sb("eg%d" % i, [128, 8, 512], BF16) for i in range(2)]
        eu = [sb("eu%d" % i, [128, 8, 512], BF16) for i in range(2)]
        ed = [sb("ed%d" % i, [128, 4, 1024], BF16) for i in range(2)]
        acc = sb("acc", [128, NT, 1024])
        hn2T = [sb("hn2T%d" % i, [128, 8, T2], BF16) for i in range(2)]
        cw = [sb("cw%d" % i, [128, NT, 16]) for i in range(2)]
        hidT = [sb("hidT%d" % i, [128, 4, 512], BF16) for i in range(2)]
        sg = [sb("sg%d" % i, [128, 512]) for i in range(2)]
        h1t = [sb("h1t%d" % i, [128, 1024]) for i in range(2)]
        xn = [sb("xn%d" % i, [128, 1024], BF16) for i in range(2)]
        junk = sb("junk", [128, 1024], BF16); junkF = sb("junkF", [128, 1024], BF16)
        ssqF = [sb("ssqF%d" % i, [128, 1]) for i in range(2)]; rstdF = [sb("rstdF%d" % i, [128, 1]) for i in range(2)]
        ssq = [sb("ssq%d" % i, [128, 1]) for i in range(2)]; rstd = [sb("rstd%d" % i, [128, 1]) for i in range(2)]
        lg = sb("lg", [128, NT, 20])
        gmax = sb("gmax", [128, NT]); oh = sb("oh", [128, NT, 4]); d4 = sb("d4", [128, NT, 4]); gsum = sb("gsum", [128, NT])
        pgr = sb("pgr", [128, NT]); ing = sb("ing", [128, NT, 4]); t4 = sb("t4", [128, NT, 4]); m1 = sb("m1", [128, NT])
        mk1 = sb("mk1", [128, NT, 4]); in2 = sb("in2", [128, NT, 4]); m2 = sb("m2", [128, NT]); mk2 = sb("mk2", [128, NT, 4])
        dm = sb("dm", [128, NT]); w1 = sb("w1", [128, NT]); w1p = sb("w1p", [128, NT]); w2p = sb("w2p", [128, NT])
        wexp = sb("wexp", [128, NT, 4])
        hn3T = [sb("hn3T%d" % i, [128, 8, 128], BF16) for i in range(2)]
        pt = [sb("pt%d" % i, [128, 256]) for i in range(2)]; pbf = [sb("pbf%d" % i, [128, 256], BF16) for i in range(2)]
        pT = [sb("pT%d" % i, [128, 2, 128], BF16) for i in range(2)]
        gsb = [sb("gsb%d" % i, [128, 512]) for i in range(2)]
        tmp = [sb("tmp%d" % i, [128, 512]) for i in range(2)]
        ot = [sb("ot%d" % i, [128, 1024]) for i in range(2)]

        S.dma('sp', identf[:], cstd[:, C_ID:C_ID + 128], w=['identf'])
        S.dma('sp', pb[:], pbd[:, :], w=['pb'])
        S.dve(lambda e: e.tensor_copy(out=ident[:], in_=identf[:]), r=['identf'], w=['ident'])
        S.dma('pool', wrt[:], w_rt.rearrange("(kc p) c -> p kc c", p=128), w=['wrt'])

        est = {'n': 0}
        total_e = nb2 * nexp

        def eload():
            n = est['n']
            if n >= total_e:
                return
            est['n'] += 1
            e_ = n % nexp; bi = n % 2
            S.dma('pool', eg[bi][:], w_g[e_].rearrange("(kc p) f -> p kc f", p=128), w=['eg%d' % bi])
            S.dma('pool', eu[bi][:], w_u[e_].rearrange("(kc p) f -> p kc f", p=128), w=['eu%d' % bi])
            S.dma('pool', ed[bi][:], w_d[e_].rearrange("(kc p) f -> p kc f", p=128), w=['ed%d' % bi])

        eload(); eload()
        S.dma('pool', wpg[:], w_pg.rearrange("(kc p) c -> p kc c", p=128), w=['wpg'])
        S.dma('pool', wpp[:], w_pp.rearrange("(kc p) c -> p kc c", p=128), w=['wpp'])

        def norm_pre(src, kx, j):
            S.act(lambda e: e.activation(out=junk[:], in_=src, func=AF.Square, accum_out=ssq[j][:]), r=[kx], w=['junk', 'ssq%d' % j])
            S.act(lambda e: e.activation(out=rstd[j][:], in_=ssq[j][:], func=AF.Sqrt, scale=1.0 / D, bias=EPS), r=['ssq%d' % j], w=['rstd%d' % j])
            S.dve(lambda e: e.reciprocal(out=rstd[j][:], in_=rstd[j][:]), r=['rstd%d' % j], w=['rstd%d' % j])
            S.pool(lambda e: e.tensor_scalar(out=xn[j][:], in0=src, scalar1=rstd[j][:, 0:1], scalar2=1.0, op0=ALU.mult, op1=ALU.mult),
                   r=[kx, 'rstd%d' % j], w=['xn%d' % j])

        def norm_pe(j):
            for kc in range(8):
                S.pe(lambda e, kc=kc: e.transpose(B0b[:, kc * 128:(kc + 1) * 128], xn[j][:, kc * 128:(kc + 1) * 128], ident[:]),
                     r=['xn%d' % j, 'ident'], w=['C0'])

        def prologue_gen(tb):
            T0 = tb * T2
            HT = hn2T[tb % 2]; kht = 'hn2T%d' % (tb % 2)
            CW = cw[tb % 2]; kcw = 'cw%d' % (tb % 2)
            def pro_pre(i):
                r0 = T0 + i * 128
                j = i % 2
                S.dma('sp', h1t[j][:], h1d[r0:r0 + 128, :], r=['h1d'], w=['h1t%d' % j])
                norm_pre(h1t[j][:], 'h1t%d' % j, j)

            pro_pre(0)
            for i in range(NT):
                j = i % 2
                if i + 1 < NT:
                    pro_pre(i + 1)
                norm_pe(j)
                S.dve(lambda e: e.tensor_tensor(out=HT[:, :, i * 128:(i + 1) * 128], in0=B0b.rearrange("p (a b) -> p a b", a=8),
                                                in1=pb[:, P_GFFN:P_GFFN + 8].unsqueeze(2).to_broadcast([128, 8, 128]), op=ALU.mult),
                      r=['C0', 'pb'], w=[kht])
                yield
                for kc in range(8):
                    S.pe(lambda e, kc=kc: e.matmul(B[1][:, 0:20], HT[:, kc, i * 128:(i + 1) * 128], wrt[:, kc, :],
                                                   start=(kc == 0), stop=(kc == 7)), r=[kht, 'wrt'], w=['C1'])
                S.dve(lambda e: e.tensor_tensor(out=lg[:, i, :], in0=B[1][:, 0:20], in1=pb[:, P_BR:P_BR + 20], op=ALU.add),
                      r=['C1', 'pb'], w=['lg'])
                yield
            bc4 = lambda a: a[:].unsqueeze(2).to_broadcast([128, NT, 4])
            S.dve(lambda e: e.tensor_reduce(out=gmax[:], in_=lg[:, :, 0:4], axis=AXX, op=ALU.max), r=['lg'], w=['gmax'])
            S.dve(lambda e: e.tensor_tensor(out=oh[:], in0=lg[:, :, 0:4], in1=bc4(gmax), op=ALU.is_equal), r=['lg', 'gmax'], w=['oh'])
            S.dve(lambda e: e.tensor_tensor(out=d4[:], in0=lg[:, :, 0:4], in1=bc4(gmax), op=ALU.subtract), r=['lg', 'gmax'], w=['d4'])
            S.act(lambda e: e.activation(out=d4[:], in_=d4[:], func=AF.Exp), r=['d4'], w=['d4'])
            S.dve(lambda e: e.tensor_reduce(out=gsum[:], in_=d4[:], axis=AXX, op=ALU.add), r=['d4'], w=['gsum'])
            S.dve(lambda e: e.reciprocal(out=pgr[:], in_=gsum[:]), r=['gsum'], w=['pgr'])
            S.dve(lambda e: e.tensor_tensor(out=ing[:], in0=lg[:, :, 4:8], in1=oh[:, :, 0:1].to_broadcast([128, NT, 4]), op=ALU.mult),
                  r=['lg', 'oh'], w=['ing'])
            for g in range(1, 4):
                S.dve(lambda e, g=g: e.tensor_tensor(out=t4[:], in0=lg[:, :, 4 + 4 * g:8 + 4 * g], in1=oh[:, :, g:g + 1].to_broadcast([128, NT, 4]),
                                                     op=ALU.mult), r=['lg', 'oh'], w=['t4'])
                S.dve(lambda e: e.tensor_tensor(out=ing[:], in0=ing[:], in1=t4[:], op=ALU.add), r=['ing', 't4'], w=['ing'])
            yield
            S.dve(lambda e: e.tensor_reduce(out=m1[:], in_=ing[:], axis=AXX, op=ALU.max), r=['ing'], w=['m1'])
            S.dve(lambda e: e.tensor_tensor(out=mk1[:], in0=ing[:], in1=bc4(m1), op=ALU.is_equal), r=['ing', 'm1'], w=['mk1'])
            S.dve(lambda e: e.scalar_tensor_tensor(out=in2[:], in0=mk1[:], scalar=NEG, in1=ing[:], op0=ALU.mult, op1=ALU.add),
                  r=['mk1', 'ing'], w=['in2'])
            S.dve(lambda e: e.tensor_reduce(out=m2[:], in_=in2[:], axis=AXX, op=ALU.max), r=['in2'], w=['m2'])
            S.dve(lambda e: e.tensor_tensor(out=mk2[:], in0=in2[:], in1=bc4(m2), op=ALU.is_equal), r=['in2', 'm2'], w=['mk2'])
            S.dve(lambda e: e.tensor_tensor(out=dm[:], in0=m2[:], in1=m1[:], op=ALU.subtract), r=['m1', 'm2'], w=['dm'])
            S.act(lambda e: e.activation(out=w1[:], in_=dm[:], func=AF.Exp), r=['dm'], w=['w1'])
            S.dve(lambda e: e.tensor_scalar(out=w1[:], in0=w1[:], scalar1=1.0, scalar2=None, op0=ALU.add), r=['w1'], w=['w1'])
            S.dve(lambda e: e.reciprocal(out=w1[:], in_=w1[:]), r=['w1'], w=['w1'])
            S.dve(lambda e: e.tensor_tensor(out=w1p[:], in0=w1[:], in1=pgr[:], op=ALU.mult), r=['w1', 'pgr'], w=['w1p'])
            S.dve(lambda e: e.tensor_tensor(out=w2p[:], in0=pgr[:], in1=w1p[:], op=ALU.subtract), r=['w1p', 'pgr'], w=['w2p'])
            yield
            S.dve(lambda e: e.tensor_tensor(out=wexp[:], in0=mk1[:], in1=bc4(w1p), op=ALU.mult), r=['mk1', 'w1p'], w=['wexp'])
            S.dve(lambda e: e.tensor_tensor(out=t4[:], in0=mk2[:], in1=bc4(w2p), op=ALU.mult), r=['mk2', 'w2p'], w=['t4'])
            S.dve(lambda e: e.tensor_tensor(out=wexp[:], in0=wexp[:], in1=t4[:], op=ALU.add), r=['wexp', 't4'], w=['wexp'])
            for g in range(4):
                S.dve(lambda e, g=g: e.tensor_tensor(out=CW[:, :, 4 * g:4 * g + 4], in0=wexp[:], in1=oh[:, :, g:g + 1].to_broadcast([128, NT, 4]),
                                                     op=ALU.mult), r=['wexp', 'oh'], w=[kcw])
            yield

        uic = [0]

        def gu(tb, e_, sub):
            HT = hn2T[tb % 2]; kht = 'hn2T%d' % (tb % 2)
            bi = (tb * nexp + e_) % 2
            G = eg[bi]; U = eu[bi]
            ts_ = slice(sub * 512, (sub + 1) * 512)
            H = hidT[sub]; kh = 'hidT%d' % sub
            for fcn in range(4):
                ui = uic[0]; uic[0] += 1
                pg = B[2 + ui % 2]; kpg = 'C%d' % (2 + ui % 2)
                pu = B[4 + ui % 2]; kpu = 'C%d' % (4 + ui % 2)
                SG = sg[ui % 2]; ksg = 'sg%d' % (ui % 2)
                for kc in range(8):
                    S.pe(lambda e, kc=kc: e.matmul(pg[:, :], G[:, kc, fcn * 128:(fcn + 1) * 128], HT[:, kc, ts_],
                                                   start=(kc == 0), stop=(kc == 7)), r=['eg%d' % bi, kht], w=[kpg])
                for kc in range(8):
                    S.pe(lambda e, kc=kc: e.matmul(pu[:, :], U[:, kc, fcn * 128:(fcn + 1) * 128], HT[:, kc, ts_],
                                                   start=(kc == 0), stop=(kc == 7)), r=['eu%d' % bi, kht], w=[kpu])
                S.act(lambda e: e.activation(out=SG[:], in_=pg[:, :], func=AF.Silu), r=[kpg], w=[ksg])
                S.dve(lambda e: e.tensor_tensor(out=H[:, fcn, :], in0=pu[:, :], in1=SG[:], op=ALU.mult), r=[kpu, ksg], w=[kh])

        def dn(tb, e_, sub):
            CW = cw[tb % 2]; kcw = 'cw%d' % (tb % 2)
            bi = (tb * nexp + e_) % 2
            Dn = ed[bi]
            H = hidT[sub]; kh = 'hidT%d' % sub
            for j in range(4):
                ti = sub * 4 + j
                for hf in range(2):
                    pd = B[6 + hf]; kpd = 'C%d' % (6 + hf)
                    cs = slice(hf * 512, (hf + 1) * 512)
                    for fcn in range(4):
                        S.pe(lambda e, fcn=fcn: e.matmul(pd[:, :], H[:, fcn, j * 128:(j + 1) * 128], Dn[:, fcn, cs],
                                                         start=(fcn == 0), stop=(fcn == 3)), r=[kh, 'ed%d' % bi], w=[kpd])
                    S.dve(lambda e: e.scalar_tensor_tensor(out=acc[:, ti, cs], in0=pd[:, :], scalar=CW[:, ti, e_:e_ + 1], in1=acc[:, ti, cs],
                                                           op0=ALU.mult, op1=ALU.add), r=[kpd, kcw, 'acc%d' % ti], w=['acc%d' % ti])

        NSUB = T2 // 512

        def experts_gen(tb, pre):
            for e_ in range(nexp):
                if e_ == 0 and pre:
                    for sub in range(NSUB):
                        dn(tb, e_, sub)
                        yield
                else:
                    for sub in range(NSUB):
                        gu(tb, e_, sub)
                        yield
                        dn(tb, e_, sub)
                        yield
                eload()

        def epilogue_gen(tb):
            T0 = tb * T2

            def epi_pre(i):
                r0 = T0 + i * 128
                j = i % 2
                norm_pre(acc[:, i, :], 'acc%d' % i, j)
                S.dma('sp', pt[j][:], p_in[r0:r0 + 128, :], w=['pt%d' % j])
                S.pool(lambda e: e.tensor_copy(out=pbf[j][:], in_=pt[j][:]), r=['pt%d' % j], w=['pbf%d' % j])

            epi_pre(0)
            for i in range(NT):
                r0 = T0 + i * 128
                ka = 'acc%d' % i
                j = i % 2
                norm_pe(j)
                S.dve(lambda e: e.tensor_tensor(out=hn3T[j][:], in0=B0b.rearrange("p (a b) -> p a b", a=8),
                                                in1=pb[:, P_GPLE:P_GPLE + 8].unsqueeze(2).to_broadcast([128, 8, 128]), op=ALU.mult),
                      r=['C0', 'pb'], w=['hn3T%d' % j])
                for c in range(2):
                    S.pe(lambda e, c=c: e.transpose(B[1][:].bitcast(BF16)[:, c * 128:(c + 1) * 128], pbf[j][:, c * 128:(c + 1) * 128], ident[:]),
                         r=['pbf%d' % j, 'ident'], w=['C1'])
                S.dve(lambda e: e.tensor_copy(out=pT[j][:], in_=B[1][:].bitcast(BF16)[:, 0:256].rearrange("p (a b) -> p a b", a=2)),
                      r=['C1'], w=['pT%d' % j])
                if i + 1 < NT:
                    epi_pre(i + 1)
                yield
                for hf in range(2):
                    cs = slice(hf * 512, (hf + 1) * 512)
                    pga = B[6]; kga = 'C6'
                    ppp = B[7]; kpp = 'C7'
                    for kc in range(8):
                        S.pe(lambda e, kc=kc: e.matmul(pga[:, :], hn3T[j][:, kc, :], wpg[:, kc, cs], start=(kc == 0), stop=(kc == 7)),
                             r=['hn3T%d' % j, 'wpg'], w=[kga])
                    for kc in range(2):
                        S.pe(lambda e, kc=kc: e.matmul(ppp[:, :], pT[j][:, kc, :], wpp[:, kc, cs], start=(kc == 0), stop=(kc == 1)),
                             r=['pT%d' % j, 'wpp'], w=[kpp])
                    S.act(lambda e: e.activation(out=gsb[hf][:], in_=pga[:, :], func=AF.Sigmoid), r=[kga], w=['gsb%d' % hf])
                    S.dve(lambda e: e.tensor_tensor(out=tmp[hf][:], in0=ppp[:, :], in1=gsb[hf][:], op=ALU.mult), r=[kpp, 'gsb%d' % hf], w=['tmp%d' % hf])
                    S.dve(lambda e: e.tensor_tensor(out=acc[:, i, cs], in0=tmp[hf][:], in1=acc[:, i, cs], op=ALU.add), r=['tmp%d' % hf, ka], w=[ka])
                S.act(lambda e: e.activation(out=junkF[:], in_=acc[:, i, :], func=AF.Square, accum_out=ssqF[j][:]), r=[ka], w=['junkF', 'ssqF%d' % j])
                S.act(lambda e: e.activation(out=rstdF[j][:], in_=ssqF[j][:], func=AF.Sqrt, scale=1.0 / D, bias=EPS), r=['ssqF%d' % j], w=['rstdF%d' % j])
                S.dve(lambda e: e.reciprocal(out=rstdF[j][:], in_=rstdF[j][:]), r=['rstdF%d' % j], w=['rstdF%d' % j])
                O = ot[j]; ko = 'ot%d' % j
                S.dve(lambda e: e.scalar_tensor_tensor(out=O[:], in0=acc[:, i, :], scalar=rstdF[j][:, 0:1], in1=pb[:, P_GFIN:P_GFIN + 1024],
                                                       op0=ALU.mult, op1=ALU.mult), r=[ka, 'rstdF%d' % j, 'pb'], w=[ko])
                S.dma('sp', out[r0:r0 + 128, :], O[:], r=[ko], w=['out'])
                if tb + 1 < nb2:
                    r1 = (tb + 1) * T2 + i * 128
                    S.dma('sp', acc[:, i, :], h1d[r1:r1 + 128, :], r=['h1d', ka], w=[ka])
                yield

        def load_acc(tb):
            for i in range(NT):
                r0 = tb * T2 + i * 128
                S.dma('sp', acc[:, i, :], h1d[r0:r0 + 128, :], r=['h1d'], w=['acc%d' % i])

        for _ in prologue_gen(0):
            pass
        load_acc(0)
        pre = False
        for tb in range(nb2):
            ge = experts_gen(tb, pre)
            gp = prologue_gen(tb + 1) if tb + 1 < nb2 else iter(())
            n_e = nexp * NSUB * 2
            n_p = 2 * NT + 3
            start_at = 4
            every = max(1, (n_e - start_at - 2) // (n_p + 1))
            k = 0
            for _ in ge:
                k += 1
                if k >= start_at and (k - start_at) % every == 0:
                    next(gp, None)
            for _ in gp:
                pass
            gepi = epilogue_gen(tb)
            if tb + 1 < nb2:
                steps = [(tb + 1, 0, sub) for sub in range(NSUB)]
                per = max(1, (2 * NT) // (len(steps) + 1))
                kk = 0
                for _ in gepi:
                    kk += 1
                    if steps and kk % per == 0:
                        gu(*steps.pop(0))
                for st in steps:
                    gu(*st)
                pre = True
            else:
                for _ in gepi:
                    pass
        S.emit()


def _consts():
    c = np.zeros((128, C_W), np.float32)
    c[:, C_ID:C_ID + 128] = np.eye(128, dtype=np.float32)
    j = np.arange(128)
    c[:, C_TRI:C_TRI + 128] = (j[:, None] <= j[None, :]).astype(np.float32)
    c[:, C_ONE:C_ONE + 128] = 1.0
    c[:, C_NEG:C_NEG + 128] = np.where(j[None, :] < j[:, None], NEG, 0.0).astype(np.float32)
    bm = np.zeros((8, 8, 128), np.float32)
    for k in range(8):
        bm[k, k, :] = 1.0
    c[0:8, C_BM:C_BM + 1024] = bm.reshape(8, 1024)
    return c


def _prep(inp):
    f = lambda a: np.ascontiguousarray(np.asarray(a, dtype=np.float32))
    L = 0
    pb = np.zeros((128, P_W), np.float32)
    bc = lambda v: np.broadcast_to(f(v).reshape(1, -1), (128, f(v).size))
    pb[:, P_FB:P_FB + 8] = bc(inp['fox_fbias'][L])
    pb[:, P_DTB:P_DTB + 8] = bc(inp['dt_bias'][L])
    pb[:, P_ALOG:P_ALOG + 8] = bc(inp['a_log'][L])
    pb[:, P_DSK:P_DSK + 8] = bc(inp['d_skip'][L])
    pb[:, P_GMIX:P_GMIX + 8] = f(inp['g_mix'][L]).reshape(8, 128).T
    pb[:, P_GFFN:P_GFFN + 8] = f(inp['g_ffn'][L]).reshape(8, 128).T
    pb[:, P_GPLE:P_GPLE + 8] = f(inp['g_ple'][L]).reshape(8, 128).T
    pb[:, P_CB:P_CB + 8] = f(inp['conv_b'][L]).reshape(8, 128).T
    cw = f(inp['conv_w'][L]).reshape(4, 8, 128)
    pb[:, P_CW:P_CW + 32] = cw.transpose(2, 1, 0).reshape(128, 32)
    pb[:, P_BR:P_BR + 4] = bc(inp['b_route_group'][L])
    pb[:, P_BR + 4:P_BR + 20] = bc(inp['b_route_expert'][L])
    pb[:, P_GSSD:P_GSSD + 512] = bc(inp['g_ssd'][L])
    pb[:, P_GFIN:P_GFIN + 1024] = bc(inp['g_final'])
    w_in = f(inp['w_in'][L])
    shared = {
        'w_in': w_in,
        'w_sm': np.ascontiguousarray(np.concatenate([w_in[:, 3080:3088], w_in[:, 1536:1544]], axis=1)),
        'w_out': f(inp['w_out'][L]),
        'w_rt': np.ascontiguousarray(np.concatenate([f(inp['w_route_group'][L]), f(inp['w_route_expert'][L])], axis=1)),
        'w_g': f(inp['w_exp_gate'][L]).reshape(16, D, 512),
        'w_u': f(inp['w_exp_up'][L]).reshape(16, D, 512),
        'w_d': f(inp['w_exp_down'][L]).reshape(16, 512, D),
        'w_pg': f(inp['w_ple_gate'][L]),
        'w_pp': f(inp['w_ple_proj'][L]),
        'pb': pb,
        'cst': _consts(),
    }
    x = f(inp['x']); p = f(inp['p'][L])
    return [dict(shared, x=x[b], p=p[b]) for b in range(8)]


def kernel(**inputs):
    stage = _CACHE.get('stage', 3)
    nc = build(stage, _CACHE.get('nb', NB), _CACHE.get('parts', 15), _CACHE.get('nb2', S_TOK // T2), _CACHE.get('nexp', 16))
    in_maps = _prep(inputs)
    res = run_bass_kernel_spmd(nc, in_maps, core_ids=list(range(8)))
    return np.stack([np.asarray(r["out"], dtype=np.float32) for r in res.results], axis=0)
```
